# Optimizing a Trainium2 kernel written in Bass

```python
import math
import jax
import jax.numpy as jnp
from jax import lax
import numpy as np

D_MODEL = 1024
BATCH = 16
SEQ = 4096
DEPTH = 2

GRID_W = 64
CTX_LEN = 256
MIX_W = D_MODEL
HEAD_DIM = 64
ATTN_W = MIX_W // 2
N_ATTN_HEADS = ATTN_W // HEAD_DIM
QK_DIM = HEAD_DIM // 2
CONV_W = MIX_W // 4
CONV_K = 3
POOL_W = MIX_W - ATTN_W - CONV_W
POOL_WINDOWS = (2, 4, 8, 16)
POOL_GC = POOL_W // len(POOL_WINDOWS)
Q_OFF = 0
K_OFF = ATTN_W
V_OFF = 2 * ATTN_W
CB_OFF = 3 * ATTN_W
CC_OFF = CB_OFF + CONV_W
CU_OFF = CC_OFF + CONV_W
POOL_OFF = CU_OFF + CONV_W
IN_W = POOL_OFF + POOL_W
ROPE_BASE = 10000.0
ROPE_PAIRS = QK_DIM // 4
N_EXPERTS = 32
TOP_K = 4
D_FF = D_MODEL
SWIGLU_ALPHA = 1.702
SWIGLU_LIMIT = 7.0
EXPERT_BLOCK = 256
Q_BLOCK = 128
N_MOD = 6
EPS = 1e-6

kernel_name = "hymba_diffattn_conv_pool_moe_dit"


def rms_norm(t, g):
    tf = t.astype(jnp.float32)
    y = tf * lax.rsqrt(jnp.mean(tf * tf, axis=-1, keepdims=True) + EPS)
    return y.astype(t.dtype) * g


def modulate(t, shift, scale):
    return t * (1 + scale) + shift


def split_heads(t, dh):
    b, l, _ = t.shape
    return t.reshape(b, l, -1, dh).transpose(0, 2, 1, 3)


def axial_rope(L):
    rows = L // GRID_W
    row = jnp.broadcast_to(jnp.arange(rows, dtype=jnp.float32)[:, None], (rows, GRID_W)).reshape(L)
    col = jnp.broadcast_to(jnp.arange(GRID_W, dtype=jnp.float32)[None, :], (rows, GRID_W)).reshape(L)
    inv = ROPE_BASE ** (-jnp.arange(ROPE_PAIRS, dtype=jnp.float32) / ROPE_PAIRS)
    ang = jnp.stack([row, col], axis=-1)[..., None] * inv
    return jnp.cos(ang), jnp.sin(ang)


def apply_rope(t, cos, sin):
    ts = t.reshape(*t.shape[:-1], 2, 2, ROPE_PAIRS)
    t1, t2 = ts[..., 0, :], ts[..., 1, :]
    cos = cos.astype(t.dtype)
    sin = sin.astype(t.dtype)
    out = jnp.stack([t1 * cos - t2 * sin, t2 * cos + t1 * sin], axis=-2)
    return out.reshape(t.shape)


def qk_pair(t, g, rope):
    t = split_heads(t, HEAD_DIM)
    t1 = rms_norm(t[..., :QK_DIM], g)
    t2 = rms_norm(t[..., QK_DIM:], g)
    if rope is not None:
        t1 = apply_rope(t1, *rope)
        t2 = apply_rope(t2, *rope)
    return t1, t2


def keys_values(pkv, g, rope):
    k1, k2 = qk_pair(pkv[..., :ATTN_W], g, rope)
    v = split_heads(pkv[..., ATTN_W:], HEAD_DIM)
    return k1, k2, v


def diff_attention(q1, q2, k1, k2, v, lam):
    b, h, lq, dq = q1.shape
    nb = lq // Q_BLOCK
    scale = dq ** -0.5

    def to_blocks(q):
        return jnp.moveaxis(q.reshape(b, h, nb, Q_BLOCK, dq), 2, 0)

    def block(qs):
        a1, a2 = qs
        s1 = jnp.einsum('bhqd,bhkd->bhqk', a1, k1).astype(jnp.float32) * scale
        s2 = jnp.einsum('bhqd,bhkd->bhqk', a2, k2).astype(jnp.float32) * scale
        pmap = jax.nn.softmax(s1, axis=-1) - lam * jax.nn.softmax(s2, axis=-1)
        return jnp.einsum('bhqk,bhkd->bhqd', pmap.astype(v.dtype), v)

    o = lax.map(block, (to_blocks(q1), to_blocks(q2)))
    return jnp.moveaxis(o, 0, 2).reshape(b, h, lq, v.shape[-1])


def short_conv(gate_b, gate_c, u, w):
    z = gate_c * u
    zp = jnp.pad(z, ((0, 0), (1, 1), (0, 0)))
    conv = w[0] * zp[:, :-2] + w[1] * zp[:, 1:-1] + w[2] * zp[:, 2:]
    return gate_b * conv


def multiscale_pool(u, w_pool, scale):
    bn, L, C = u.shape
    cs = jnp.cumsum(u.astype(jnp.float32), axis=1)
    S = jnp.concatenate([jnp.zeros((bn, 1, C), jnp.float32), cs], axis=1)
    t = jnp.arange(L)
    groups = []
    for g, win in enumerate(POOL_WINDOWS):
        half = win // 2
        lo = jnp.clip(t - half, 0, L)
        hi = jnp.clip(t + half, 0, L)
        Sg = S[..., g * POOL_GC:(g + 1) * POOL_GC]
        mean = (Sg[:, hi] - Sg[:, lo]) / (hi - lo).astype(jnp.float32)[:, None]
        groups.append(mean - u[..., g * POOL_GC:(g + 1) * POOL_GC].astype(jnp.float32))
    d = jnp.stack(groups, axis=2).astype(u.dtype)
    y = jnp.einsum('blgc,gcd->blgd', d, w_pool).reshape(bn, L, POOL_W)
    return y * scale


def mix_tokens(p, keys, rope, lam, lam_init, q_g, subln_g, conv_w, pool_w, pool_scale, w_out):
    bn, L, _ = p.shape
    q1, q2 = qk_pair(p[..., Q_OFF:K_OFF], q_g, rope)
    k1, k2, v = keys
    o = diff_attention(q1, q2, k1, k2, v, lam)
    o = rms_norm(o, subln_g) * (1.0 - lam_init)
    o_att = o.transpose(0, 2, 1, 3).reshape(bn, L, ATTN_W)
    o_conv = short_conv(p[..., CB_OFF:CC_OFF], p[..., CC_OFF:CU_OFF], p[..., CU_OFF:POOL_OFF], conv_w)
    o_pool = multiscale_pool(p[..., POOL_OFF:IN_W], pool_w, pool_scale)
    return jnp.concatenate([o_att, o_conv, o_pool], axis=-1) @ w_out


def moe_ffn(h, w_r, b_r, w_gu, b_gu, w_dn, b_dn):
    T, D = h.shape
    logits = (h @ w_r).astype(jnp.float32) + b_r.astype(jnp.float32)
    top_logit, top_e = lax.top_k(logits, TOP_K)
    gate_w = jax.nn.softmax(top_logit, axis=-1)
    n_assign = T * TOP_K
    flat_e = top_e.reshape(-1)
    order = jnp.argsort(flat_e)
    e_sorted = flat_e[order]
    tok_sorted = (order // TOP_K).astype(jnp.int32)
    w_sorted = gate_w.reshape(-1)[order]
    counts = jax.ops.segment_sum(jnp.ones_like(flat_e), flat_e, num_segments=N_EXPERTS)
    padded = (counts + EXPERT_BLOCK - 1) // EXPERT_BLOCK * EXPERT_BLOCK
    pad_end = jnp.cumsum(padded)
    pad_start = pad_end - padded
    start = jnp.cumsum(counts) - counts
    dest = pad_start[e_sorted] + (jnp.arange(n_assign) - start[e_sorted])
    n_blocks = -(-(n_assign + N_EXPERTS * (EXPERT_BLOCK - 1)) // EXPERT_BLOCK)
    n_rows = n_blocks * EXPERT_BLOCK
    row_tok = jnp.full((n_rows,), T, jnp.int32).at[dest].set(tok_sorted)
    row_w = jnp.zeros((n_rows,), jnp.float32).at[dest].set(w_sorted)
    block_e = jnp.minimum(jnp.searchsorted(pad_end, jnp.arange(n_blocks) * EXPERT_BLOCK, side='right'),
                          N_EXPERTS - 1)
    h_pad = jnp.concatenate([h, jnp.zeros((1, D), h.dtype)], axis=0)

    def expert_block(args):
        toks, wts, e = args
        xb = h_pad[toks]
        gu = xb @ w_gu[e] + b_gu[e]
        glu = jnp.minimum(gu[:, :D_FF], SWIGLU_LIMIT)
        lin = jnp.clip(gu[:, D_FF:], -SWIGLU_LIMIT, SWIGLU_LIMIT)
        act = glu * jax.nn.sigmoid(SWIGLU_ALPHA * glu) * (lin + 1)
        y = act @ w_dn[e] + b_dn[e]
        return y * wts[:, None].astype(y.dtype)

    y = lax.map(expert_block, (row_tok.reshape(n_blocks, EXPERT_BLOCK),
                               row_w.reshape(n_blocks, EXPERT_BLOCK), block_e))
    return jax.ops.segment_sum(y.reshape(n_rows, D), row_tok, num_segments=T + 1)[:T]


def setup_inputs(seed: int = 0) -> dict:
    key = jax.random.key(seed)
    ks = jax.random.split(key, 26)
    f32 = jnp.float32

    def nrm(k, shape, s):
        return jax.random.normal(k, shape, f32) * s

    return {
        "x": nrm(ks[0], (BATCH, SEQ, D_MODEL), 1.0),
        "c": nrm(ks[1], (BATCH, D_MODEL), 1.0),
        "ctx": nrm(ks[2], (BATCH, CTX_LEN, D_MODEL), 1.0),
        "c_ctx": nrm(ks[3], (D_MODEL,), 1.0),
        "w_mod": nrm(ks[4], (DEPTH, D_MODEL, N_MOD * D_MODEL), 0.3 * D_MODEL ** -0.5),
        "b_mod": nrm(ks[5], (DEPTH, N_MOD * D_MODEL), 0.02),
        "norm1_g": 1.0 + nrm(ks[6], (DEPTH, D_MODEL), 0.05),
        "norm2_g": 1.0 + nrm(ks[7], (DEPTH, D_MODEL), 0.05),
        "w_in": nrm(ks[8], (DEPTH, D_MODEL, IN_W), D_MODEL ** -0.5),
        "q_norm_g": 1.0 + nrm(ks[9], (DEPTH, QK_DIM), 0.05),
        "k_norm_g": 1.0 + nrm(ks[10], (DEPTH, QK_DIM), 0.05),
        "lambda_q1": nrm(ks[11], (DEPTH, QK_DIM), 0.1),
        "lambda_k1": nrm(ks[12], (DEPTH, QK_DIM), 0.1),
        "lambda_q2": nrm(ks[13], (DEPTH, QK_DIM), 0.1),
        "lambda_k2": nrm(ks[14], (DEPTH, QK_DIM), 0.1),
        "subln_g": 1.0 + nrm(ks[15], (DEPTH, HEAD_DIM), 0.05),
        "conv_w": nrm(ks[16], (DEPTH, CONV_K, CONV_W), CONV_K ** -0.5),
        "pool_w": nrm(ks[17], (DEPTH, len(POOL_WINDOWS), POOL_GC, POOL_GC), POOL_GC ** -0.5),
        "pool_scale": 1.0 + nrm(ks[18], (DEPTH, POOL_W), 0.1),
        "w_out": nrm(ks[19], (DEPTH, MIX_W, D_MODEL), MIX_W ** -0.5),
        "router_w": nrm(ks[20], (DEPTH, D_MODEL, N_EXPERTS), D_MODEL ** -0.5),
        "router_b": nrm(ks[21], (DEPTH, N_EXPERTS), 0.01),
        "w_gate_up": nrm(ks[22], (DEPTH, N_EXPERTS, D_MODEL, 2 * D_FF), D_MODEL ** -0.5),
        "b_gate_up": nrm(ks[23], (DEPTH, N_EXPERTS, 2 * D_FF), 0.02),
        "w_down": nrm(ks[24], (DEPTH, N_EXPERTS, D_FF, D_MODEL), D_FF ** -0.5),
        "b_down": nrm(ks[25], (DEPTH, N_EXPERTS, D_MODEL), 0.02),
    }


def reference(x, c, ctx, c_ctx, w_mod, b_mod, norm1_g, norm2_g, w_in, q_norm_g, k_norm_g,
              lambda_q1, lambda_k1, lambda_q2, lambda_k2, subln_g, conv_w, pool_w, pool_scale, w_out,
              router_w, router_b, w_gate_up, b_gate_up, w_down, b_down):
    B, L, D = x.shape
    rope = axial_rope(L)
    f32 = jnp.float32
    for l in range(DEPTH):
        has_next = l + 1 < DEPTH
        lam_init = 0.8 - 0.6 * math.exp(-0.3 * l)
        lam = (jnp.exp(jnp.sum(lambda_q1[l].astype(f32) * lambda_k1[l].astype(f32)))
               - jnp.exp(jnp.sum(lambda_q2[l].astype(f32) * lambda_k2[l].astype(f32))) + lam_init)
        mod = (jax.nn.silu(c) @ w_mod[l] + b_mod[l]).reshape(B, 1, N_MOD, D)
        mod_c = (jax.nn.silu(c_ctx) @ w_mod[l] + b_mod[l]).reshape(N_MOD, D)

        h = modulate(rms_norm(x, norm1_g[l]), mod[:, :, 0], mod[:, :, 1])
        hc = modulate(rms_norm(ctx, norm1_g[l]), mod_c[0], mod_c[1])
        p = h @ w_in[l]
        if has_next:
            pc = hc @ w_in[l]
            pc_kv = pc[..., K_OFF:CB_OFF]
        else:
            pc_kv = hc @ w_in[l][:, K_OFF:CB_OFF]
        ck1, ck2, cv = keys_values(pc_kv, k_norm_g[l], None)
        k1, k2, v = keys_values(p[..., K_OFF:CB_OFF], k_norm_g[l], rope)
        lat_keys = (jnp.concatenate([ck1, k1], axis=2),
                    jnp.concatenate([ck2, k2], axis=2),
                    jnp.concatenate([cv, v], axis=2))
        layer_w = (q_norm_g[l], subln_g[l], conv_w[l], pool_w[l], pool_scale[l], w_out[l])
        x = x + mod[:, :, 2] * mix_tokens(p, lat_keys, rope, lam, lam_init, *layer_w)
        if has_next:
            ctx = ctx + mod_c[2] * mix_tokens(pc, (ck1, ck2, cv), None, lam, lam_init, *layer_w)

        moe_w = (router_w[l], router_b[l], w_gate_up[l], b_gate_up[l], w_down[l], b_down[l])
        h2 = modulate(rms_norm(x, norm2_g[l]), mod[:, :, 3], mod[:, :, 4])
        if has_next:
            h2c = modulate(rms_norm(ctx, norm2_g[l]), mod_c[3], mod_c[4])
            n_c = h2c.shape[0] * h2c.shape[1]
            y = moe_ffn(jnp.concatenate([h2c.reshape(-1, D), h2.reshape(-1, D)], axis=0), *moe_w)
            ctx = ctx + mod_c[5] * y[:n_c].reshape(ctx.shape)
            x = x + mod[:, :, 5] * y[n_c:].reshape(x.shape)
        else:
            x = x + mod[:, :, 5] * moe_ffn(h2.reshape(-1, D), *moe_w).reshape(x.shape)
    return x
```

```python
import math
import numpy as np
import ml_dtypes
import concourse.bass as bass
import concourse.mybir as mybir
from concourse.bass_utils import run_bass_kernel_spmd

F32 = mybir.dt.float32
BF16 = mybir.dt.bfloat16
AF = mybir.ActivationFunctionType
ALU = mybir.AluOpType
AX = mybir.AxisListType

D = 1024
L = 4096
CL = 256
NCORES = 8
NSEQ = 2
DEPTH = 2
IN_W = 2560
NE = 32
EPS = 1e-6
TG = 1024


class Buf:
    __slots__ = ("name", "last_w", "readers", "sem", "cnt", "excl")

    def __init__(self, name, excl=False):
        self.name = name
        self.excl = excl
        self.last_w = None
        self.readers = {}
        self.sem = None
        self.cnt = 0


class Ins:
    __slots__ = ("eng", "fn", "deps", "need_inc", "ev", "dma_buf", "snap")

    def __init__(self, eng, fn, dma_buf=None):
        self.eng = eng
        self.fn = fn
        self.deps = []
        self.need_inc = False
        self.ev = None
        self.dma_buf = dma_buf
        self.snap = None


class Tracker:
    ENGS = ("pe", "act", "dve", "pool", "sp")

    def __init__(self):
        self.prog = []
        self.last_on = {e: None for e in self.ENGS}
        self.dma_bufs = []

    def add(self, eng, fn, r=(), w=(), dma_buf=None):
        ins = Ins(eng, fn, dma_buf)
        if any(b.excl for b in r):
            w = tuple(w) + tuple(b for b in r if b.excl)
            r = tuple(b for b in r if not b.excl)
        deps = {}
        for b in r:
            if b.last_w is not None:
                deps[id(b.last_w)] = b.last_w
        for b in w:
            if b.last_w is not None:
                deps[id(b.last_w)] = b.last_w
            for rd in b.readers.values():
                deps[id(rd)] = rd
        deps.pop(id(ins), None)
        for d in deps.values():
            if d.eng == "pe" and eng == "pe" and d.dma_buf is None and dma_buf is None:
                continue
            ins.deps.append(d)
            d.need_inc = True
        key = eng if dma_buf is None else ("dma", id(dma_buf))
        for b in r:
            b.readers[key] = ins
        for b in w:
            b.last_w = ins
            b.readers = {}
        if dma_buf is not None and dma_buf.sem is None:
            dma_buf.sem = "pending"
            self.dma_bufs.append(dma_buf)
        self.prog.append(ins)
        if dma_buf is None:
            self.last_on[eng] = ins
        return ins

    def barrier(self):
        for e in self.ENGS:
            li = self.last_on[e]
            if li is not None:
                li.need_inc = True
        self.prog.append("barrier")

    def mm(self, out, lhsT, rhs, start, stop, r, w, **kw):
        return self.add("pe", lambda e: e.matmul(out, lhsT, rhs, start=start, stop=stop, **kw), r, w)

    def tr(self, out, in_, ident, r, w):
        return self.add("pe", lambda e: e.transpose(out, in_, ident), r, w)

    def act(self, out, in_, func, r, w, **kw):
        return self.add("act", lambda e: e.activation(out=out, in_=in_, func=func, **kw), r, w)

    def ts(self, out, in0, s1, s2, op0, r, w, op1=None, eng="dve"):
        if op1 is None:
            return self.add(eng, lambda e: e.tensor_scalar(out, in0, s1, None, op0), r, w)
        return self.add(eng, lambda e: e.tensor_scalar(out, in0, s1, s2, op0, op1), r, w)

    def tt(self, out, in0, in1, op, r, w, eng="dve"):
        return self.add(eng, lambda e: e.tensor_tensor(out, in0, in1, op), r, w)

    def stt(self, out, in0, scalar, in1, op0, op1, r, w, eng="dve", **kw):
        return self.add(eng, lambda e: e.scalar_tensor_tensor(out, in0, scalar, in1, op0, op1, **kw), r, w)

    def copy(self, out, in_, r, w, eng="dve"):
        return self.add(eng, lambda e: e.tensor_copy(out, in_), r, w)

    def recip(self, out, in_, r, w):
        return self.add("dve", lambda e: e.reciprocal(out, in_), r, w)

    def rsum(self, out, in_, r, w):
        return self.add("dve", lambda e: e.reduce_sum(out, in_, AX.X), r, w)

    def max8(self, out, in_, r, w):
        return self.add("dve", lambda e: e.max(out=out, in_=in_), r, w)

    def memset(self, ap, val, w, eng="dve"):
        return self.add(eng, lambda e: e.memset(ap, val), (), w)

    def dma(self, eng, out, in_, buf, r=(), w=()):
        return self.add(eng, lambda e: e.dma_start(out=out, in_=in_), r, w, dma_buf=buf)

    def load(self, eng, out, in_, buf):
        return self.dma(eng, out, in_, buf, r=(), w=(buf,))

    def store(self, eng, out, in_, buf):
        return self.dma(eng, out, in_, buf, r=(buf,), w=())

    def emit(self, nc):
        engsem = {e: nc.alloc_semaphore("sem_" + e) for e in self.ENGS}
        cnt = {e: 0 for e in self.ENGS}
        for b in self.dma_bufs:
            b.sem = nc.alloc_semaphore("dsem_" + b.name)
        lists = {e: [] for e in self.ENGS}
        for ins in self.prog:
            if ins == "barrier":
                snap = [(engsem[e], cnt[e]) for e in self.ENGS if cnt[e] > 0]
                snap += [(b.sem, b.cnt) for b in self.dma_bufs if b.cnt > 0]
                for e in self.ENGS:
                    lists[e].append(("barrier", snap))
                continue
            if ins.dma_buf is not None:
                b = ins.dma_buf
                b.cnt += 16
                ins.ev = (b.sem, b.cnt)
            elif ins.need_inc:
                cnt[ins.eng] += 1
                ins.ev = (engsem[ins.eng], cnt[ins.eng])
            lists[ins.eng].append(ins)
        final = [(engsem[e], cnt[e]) for e in self.ENGS if cnt[e] > 0]
        final += [(b.sem, b.cnt) for b in self.dma_bufs if b.cnt > 0]
        self.n_ins = {e: len(lists[e]) for e in self.ENGS}

        def replay(e, name):
            waited = {}

            def wait(sem, val):
                k = id(sem)
                if waited.get(k, 0) < val:
                    e.wait_ge(sem, val)
                    waited[k] = val

            for ins in lists[name]:
                if isinstance(ins, tuple):
                    for sem, val in ins[1]:
                        wait(sem, val)
                    continue
                for d in ins.deps:
                    wait(*d.ev)
                bi = ins.fn(e)
                if ins.dma_buf is not None:
                    bi.then_inc(ins.ev[0], 16)
                elif ins.need_inc:
                    bi.then_inc(ins.ev[0], 1)
            if name == "sp":
                for sem, val in final:
                    wait(sem, val)

        with nc.Block() as block:
            @block.tensor
            def _(e):
                replay(e, "pe")

            @block.scalar
            def _(e):
                replay(e, "act")

            @block.vector
            def _(e):
                replay(e, "dve")

            @block.gpsimd
            def _(e):
                replay(e, "pool")

            @block.sync
            def _(e):
                replay(e, "sp")


class Alloc:
    def __init__(self, nc, limit=229376 - 256):
        self.nc = nc
        self.off = 16640
        self.limit = limit
        self.n = 0
        self.peak = 0
        self.bufs = {}
        self.handles = {}

    def t(self, name, shape, dtype):
        size = 1
        for s in shape[1:]:
            size *= s
        size *= 2 if dtype == BF16 else 4
        off = (self.off + 63) // 64 * 64
        assert off + size <= self.limit, f"SBUF overflow at {name}: {off + size}"
        key = (name, off, tuple(shape), str(dtype))
        if key in self.handles:
            h = self.handles[key]
        else:
            self.n += 1
            h = self.nc.alloc_sbuf_tensor_at(f"{name}_{self.n}", list(shape), dtype, offset=off)
            self.handles[key] = h
        self.off = off + size
        self.peak = max(self.peak, self.off)
        if name not in self.bufs:
            self.bufs[name] = Buf(name)
        return h, self.bufs[name]

    def mark(self):
        return self.off

    def reset(self, m):
        self.off = m


def _consts():
    p = np.arange(128)
    i = p % 32
    axis = i // 16
    half = (i % 16) // 8
    pair = i % 8
    t = np.arange(L)
    row = (t // 64).astype(np.float32)
    col = (t % 64).astype(np.float32)
    inv = (np.float32(10000.0) ** (-np.arange(8, dtype=np.float32) / np.float32(8))).astype(np.float32)
    pos = np.where(axis[:, None] == 0, row[None, :], col[None, :]).astype(np.float32)
    ang = (pos * inv[pair][:, None]).astype(np.float32)
    ropeC = np.cos(ang).astype(np.float32)
    ropeS = (np.sin(ang) * np.where(half == 0, -1.0, 1.0)[:, None]).astype(np.float32)
    partner = np.where(half == 0, p + 8, p - 8)
    perm = np.zeros((128, 128), np.float32)
    perm[partner, p] = 1.0
    blk = (p[:, None] // 32 == p[None, :] // 32).astype(np.float32) / 32.0
    ident = np.eye(128, dtype=np.float32)
    wins = (2, 4, 8, 16)
    pwinv = np.zeros((128, 2), np.float32)
    pedge = np.zeros((128, 2, 16), np.float32)
    for c in range(2):
        for gl in range(2):
            w = wins[2 * c + gl]
            hf = w // 2
            sl = slice(gl * 64, gl * 64 + 64)
            pwinv[sl, c] = 1.0 / w
            for j in range(8):
                pedge[sl, c, j] = 1.0 / (min(j + hf, 1 << 30) - max(j - hf, 0))
                dist = 8 - j
                pedge[sl, c, 8 + j] = 1.0 / (min(hf, dist) + hf)
    sel = np.zeros((32, 32, 128), np.float32)
    return dict(ropeC=ropeC, ropeS=ropeS, perm=perm, blk=blk, ident=ident, pwinv=pwinv, pedge=pedge)


def _fm(v):
    sh = v.shape
    n = sh[-1] // 128
    return np.ascontiguousarray(np.swapaxes(v.reshape(*sh[:-1], n, 128), -1, -2))


def _shared_inputs(I):
    f = np.float32
    S = {}
    S["w_mod"] = np.ascontiguousarray(I["w_mod"], dtype=f)
    S["bmodx"] = np.ascontiguousarray(np.repeat(_fm(I["b_mod"])[..., None], 3, axis=-1), dtype=f)
    S["n1gx"] = np.ascontiguousarray(np.repeat(_fm(I["norm1_g"])[..., None], 3, axis=-1), dtype=f)
    S["n2gx"] = np.ascontiguousarray(np.repeat(_fm(I["norm2_g"])[..., None], 3, axis=-1), dtype=f)
    S["w_in"] = np.ascontiguousarray(I["w_in"], dtype=f)
    S["w_out"] = np.ascontiguousarray(I["w_out"], dtype=f)
    qk = np.stack([np.tile(I["q_norm_g"], (1, 4)), np.tile(I["k_norm_g"], (1, 4))], axis=-1)
    S["qkg"] = np.ascontiguousarray(qk, dtype=f)
    S["lam4"] = np.ascontiguousarray(np.stack([I["lambda_q1"], I["lambda_k1"], I["lambda_q2"], I["lambda_k2"]], axis=1), dtype=f)
    S["subg2"] = np.ascontiguousarray(np.tile(I["subln_g"], (1, 2)), dtype=f)
    cw = I["conv_w"]
    S["convw"] = np.ascontiguousarray(np.transpose(cw.reshape(2, 3, 2, 128), (0, 3, 2, 1)), dtype=f)
    pw = I["pool_w"]
    bd = np.zeros((2, 2, 128, 128), f)
    for l in range(2):
        for c in range(2):
            for gl in range(2):
                bd[l, c, gl * 64:(gl + 1) * 64, gl * 64:(gl + 1) * 64] = pw[l, 2 * c + gl]
    S["poolbd"] = bd
    S["pools"] = np.ascontiguousarray(_fm(I["pool_scale"]), dtype=f)
    S["router_w"] = np.ascontiguousarray(I["router_w"], dtype=f)
    S["router_b"] = np.ascontiguousarray(I["router_b"], dtype=f)
    S["w_gate_up"] = np.ascontiguousarray(I["w_gate_up"], dtype=f)
    S["w_down"] = np.ascontiguousarray(I["w_down"], dtype=f)
    S["bguT"] = np.ascontiguousarray(np.transpose(_fm(I["b_gate_up"]), (0, 2, 1, 3)), dtype=f)
    S["b_down"] = np.ascontiguousarray(I["b_down"], dtype=f)
    S.update(_consts())
    return S


def _core_inputs(I, core):
    b0 = NSEQ * core
    f = np.float32
    C = {}
    C["xT"] = np.ascontiguousarray(np.transpose(I["x"][b0:b0 + NSEQ], (0, 2, 1)), dtype=f)
    C["ctxT"] = np.ascontiguousarray(np.transpose(I["ctx"][b0:b0 + NSEQ], (0, 2, 1)), dtype=f)
    cc = np.stack([I["c"][b0], I["c"][b0 + 1], I["c_ctx"]], axis=-1)
    C["ccT"] = np.ascontiguousarray(np.transpose(cc.reshape(8, 128, 3), (1, 0, 2)), dtype=f)
    return C


IN_SHAPES = dict(
    xT=[NSEQ, D, L], ctxT=[NSEQ, D, CL], ccT=[128, 8, 3],
    w_mod=[2, D, 6 * D], bmodx=[2, 128, 48, 3], n1gx=[2, 128, 8, 3], n2gx=[2, 128, 8, 3],
    w_in=[2, D, IN_W], w_out=[2, D, D], qkg=[2, 128, 2], lam4=[2, 4, 32], subg2=[2, 128],
    convw=[2, 128, 2, 3], poolbd=[2, 2, 128, 128], pools=[2, 128, 2],
    router_w=[2, D, NE], router_b=[2, NE], w_gate_up=[2, NE, D, 2 * D], w_down=[2, NE, D, D],
    bguT=[2, 128, NE, 16], b_down=[2, NE, D],
    ropeC=[128, L], ropeS=[128, L], perm=[128, 128], blk=[128, 128], ident=[128, 128],
    pwinv=[128, 2], pedge=[128, 2, 16],
)


def build(depth=DEPTH, dbg=False, stop=None, rowtile=True, p1cut=4, p1chunks=None, p2cut=6, p2chunks=None, ne_run=NE, stop_layer=0):
    nc = bass.Bass("TRN2", target_bir_lowering=False)
    T = Tracker()
    A = Alloc(nc)
    class _LazyIn(dict):
        def __missing__(self, k):
            v = nc.dram_tensor(k, IN_SHAPES[k], F32, kind="ExternalInput").ap()
            self[k] = v
            return v

    IN = _LazyIn()
    out = nc.dram_tensor("outT", [NSEQ, D, L], F32, kind="ExternalOutput").ap()
    skind = "ExternalOutput" if dbg else "Internal"

    def scratch(name, shape, dt=F32):
        return nc.dram_tensor(name, shape, dt, kind=skind).ap()

    x1res = scratch("x1res", [NSEQ, D, L])
    x2res = scratch("x2res", [NSEQ, D, L])
    c1res = scratch("c1res", [NSEQ, D, CL])
    c2res = scratch("c2res", [NSEQ, D, CL])
    stash = scratch("stash", [NSEQ, 6, 128, CL + L])
    TT = NSEQ * L + NSEQ * CL
    h2T = scratch("h2T", [D, TT], BF16)
    GT = scratch("GT", [NE, TT])

    def dump(name, ap, buf, shape, dt=F32):
        if dbg:
            d_ = nc.dram_tensor("dbg_" + name, list(shape), dt, kind="ExternalOutput").ap()
            T.store("sp", d_, ap, buf)

    ps = [nc.alloc_psum_tensor(f"ps{i}", [128, 512], F32) for i in range(8)]
    PS = [Buf(f"ps{i}", excl=True) for i in range(8)]

    ones_bf, B_ones = A.t("ones", [128, 128], BF16)
    blk_bf, B_blk = A.t("blk", [128, 128], BF16)
    perm_bf, B_perm = A.t("perm", [128, 128], BF16)
    ident, B_ident = A.t("ident", [128, 128], F32)
    sc, B_sc = A.t("sc", [128, 8, 3], F32)
    modT, B_mod = A.t("modT", [128, 48, 3], F32)
    gm, B_gm = A.t("gm", [128, 16, 3], F32)
    bmodx, B_bmodx = A.t("bmodx", [128, 48, 3], F32)
    ngx, B_ngx = A.t("ngx", [128, 16, 3], F32)
    qkg, B_qkg = A.t("qkg", [128, 2], F32)
    qkgs, B_qkgs = A.t("qkgs", [128, 2], F32)
    lam4b, B_lam4 = A.t("lam4b", [128, 4, 32], F32)
    lamt, B_lamt = A.t("lamt", [128, 8], F32)
    subgb, B_subg = A.t("subgb", [128, 128], F32)
    convw, B_convw = A.t("convw", [128, 2, 3], F32)
    pools, B_pools = A.t("pools", [128, 2], F32)
    pwinv, B_pwinv = A.t("pwinv", [128, 2], F32)
    pedge, B_pedge = A.t("pedge", [128, 2, 16], F32)
    poolbd, B_poolbd = A.t("poolbd", [128, 2, 128], BF16)
    wr, B_wr = A.t("wr", [128, 8, NE], F32)
    rbb, B_rbb = A.t("rbb", [128, NE], F32)
    bgu, B_bgu = A.t("bgu", [128, NE, 16], F32)
    bdn, B_bdn = A.t("bdn", [NE, D], F32)

    epsb, B_epsb = A.t("epsb", [128, 2], F32)
    T.memset(epsb[:, 0:1], EPS, (B_epsb,))
    T.memset(epsb[:, 1:2], 64.0 * EPS, (B_epsb,))
    T.memset(ones_bf[:], 1.0 / 1024.0, (B_ones,))
    T.load("pool", blk_bf[:], IN["blk"], B_blk)
    T.load("pool", perm_bf[:], IN["perm"], B_perm)
    T.load("sp", ident[:], IN["ident"], B_ident)
    T.load("sp", sc[:], IN["ccT"], B_sc)
    T.load("sp", pwinv[:], IN["pwinv"], B_pwinv)
    T.load("sp", pedge[:], IN["pedge"], B_pedge)
    T.act(sc[:], sc[:], AF.Silu, (B_sc,), (B_sc,))
    base_mark = A.mark()

    CHUNKS = [(True, 0, CL, 0)] + [(False, 512 * i, 512, CL + 512 * i) for i in range(L // 512)]

    stop_ = stop
    for l in range(depth):
        stop = stop_ if l == stop_layer else None
        last = (l == DEPTH - 1)
        lam_init = 0.8 - 0.6 * math.exp(-0.3 * l)
        xin = IN["xT"] if l == 0 else x2res
        cin = IN["ctxT"] if l == 0 else c2res
        xout = x2res

        A.reset(base_mark)
        T.load("sp", bmodx[:], IN["bmodx"][l], B_bmodx)
        T.load("sp", ngx[:, 0:8, :], IN["n1gx"][l], B_ngx)
        T.load("sp", ngx[:, 8:16, :], IN["n2gx"][l], B_ngx)
        T.load("sp", qkg[:], IN["qkg"][l], B_qkg)
        T.load("sp", lam4b[:], IN["lam4"][l].partition_broadcast(128), B_lam4)
        T.load("sp", subgb[:], IN["subg2"][l].partition_broadcast(128), B_subg)
        T.load("sp", convw[:], IN["convw"][l], B_convw)
        T.load("sp", pools[:], IN["pools"][l], B_pools)
        T.load("pool", poolbd[:], IN["poolbd"][l].rearrange("c p j -> p c j"), B_poolbd)
        T.load("sp", wr[:], IN["router_w"][l].rearrange("(k p) e -> p k e", p=128), B_wr)
        T.load("sp", rbb[:], IN["router_b"][l].partition_broadcast(128), B_rbb)
        T.load("sp", bgu[:], IN["bguT"][l], B_bgu)
        T.load("sp", bdn[:], IN["b_down"][l], B_bdn)
        T.ts(bgu[:, :, 8:16], bgu[:, :, 8:16], 1.0, None, ALU.add, (B_bgu,), (B_bgu,))
        T.ts(qkgs[:, 0:1], qkg[:, 0:1], float(32 ** -0.5), None, ALU.mult, (B_qkg,), (B_qkgs,))
        T.copy(qkgs[:, 1:2], qkg[:, 1:2], (B_qkg,), (B_qkgs,))
        T.ts(subgb[:], subgb[:], float(8.0 * (1.0 - lam_init)), None, ALU.mult, (B_subg,), (B_subg,))
        T.memset(lamt[:], 0.0, (B_lamt,))
        T.tt(lam4b[:, 0, :], lam4b[:, 0, :], lam4b[:, 1, :], ALU.mult, (B_lam4,), (B_lam4,))
        T.tt(lam4b[:, 2, :], lam4b[:, 2, :], lam4b[:, 3, :], ALU.mult, (B_lam4,), (B_lam4,))
        T.rsum(lamt[:, 0:1], lam4b[:, 0, :], (B_lam4,), (B_lamt,))
        T.rsum(lamt[:, 1:2], lam4b[:, 2, :], (B_lam4,), (B_lamt,))
        T.act(lamt[:, 2:4], lamt[:, 0:2], AF.Exp, (B_lamt,), (B_lamt,))
        T.tt(lamt[:, 4:5], lamt[:, 3:4], lamt[:, 2:3], ALU.subtract, (B_lamt,), (B_lamt,))
        T.ts(lamt[:, 5:6], lamt[:, 4:5], float(-lam_init), None, ALU.add, (B_lamt,), (B_lamt,))
        nlam = lamt[:, 5:6]

        m0 = A.mark()
        wm = [A.t(f"wm{i}", [128, 8, 768], F32) for i in range(2)]
        for jg in range(8):
            wt, wb = wm[jg % 2]
            T.load("sp", wt[:], IN["w_mod"][l, :, jg * 768:(jg + 1) * 768].rearrange("(k p) j -> p k j", p=128), wb)
            for jj in range(6):
                j = jg * 6 + jj
                for k in range(8):
                    T.mm(ps[0][:, j * 3:(j + 1) * 3], wt[:, k, jj * 128:(jj + 1) * 128], sc[:, k, :],
                         k == 0, k == 7, (wb, B_sc), (PS[0],))
        T.tt(modT[:].rearrange("p j c -> p (j c)"), ps[0][:, 0:144], bmodx[:].rearrange("p j c -> p (j c)"),
             ALU.add, (PS[0], B_bmodx), (B_mod,))
        T.stt(gm[:, 0:8, :], modT[:, 8:16, :], 1.0, ngx[:, 0:8, :], ALU.add, ALU.mult, (B_mod, B_ngx), (B_gm,))
        T.stt(gm[:, 8:16, :], modT[:, 32:40, :], 1.0, ngx[:, 8:16, :], ALU.add, ALU.mult, (B_mod, B_ngx), (B_gm,))
        A.reset(m0)
        dump(f"modT{l}", modT[:], B_mod, [128, 48, 3])
        dump(f"gm{l}", gm[:], B_gm, [128, 16, 3])
        dump(f"lamt{l}", lamt[:], B_lamt, [128, 8])
        if stop == "mod":
            break

        def rms_modulate(xc, B_xc, N, col, which, outs, B_out, sq, B_sq, rstd, B_rstd, tmp, B_tmp, ssb):
            go = 0 if which == 1 else 8
            so = 0 if which == 1 else 24
            for k in range(8):
                q, bq = sq[k % 2], B_sq[k % 2]
                T.act(q[:, 0:N], xc[:, k, 0:N], AF.Square, (B_xc,), (bq,))
                T.mm(ps[ssb][:, 0:N], ones_bf[:], q[:, 0:N], k == 0, k == 7, (B_ones, bq), (PS[ssb],))
            T.act(rstd[:, 0:N], ps[ssb][:, 0:N], AF.Sqrt, (PS[ssb], B_epsb), (B_rstd,), bias=epsb[:, 0:1], scale=1.0)
            T.recip(rstd[:, 0:N], rstd[:, 0:N], (B_rstd,), (B_rstd,))
            for k in range(8):
                t_, bt = tmp[k % 2], B_tmp[k % 2]
                T.stt(t_[:, 0:N], xc[:, k, 0:N], gm[:, go + k, col:col + 1], rstd[:, 0:N], ALU.mult, ALU.mult,
                      (B_xc, B_gm, B_rstd), (bt,))
                T.act(outs[k], t_[:, 0:N], AF.Identity, (bt, B_mod), (B_out,), bias=modT[:, so + k, col:col + 1], scale=1.0)

        def qk_post(psb, N, gcol, is_ctx, t0, dst, B_dst, W):
            sqk, B_sqk, ug, B_ug, rstd2, B_rstd2, t1, B_t1, t2, B_t2, ropc, rops, B_rope = W
            T.act(sqk[:, 0:N], ps[psb][:, 0:N], AF.Square, (PS[psb],), (B_sqk,))
            T.ts(ug[:, 0:N], ps[psb][:, 0:N], qkgs[:, gcol:gcol + 1], None, ALU.mult, (PS[psb], B_qkgs), (B_ug,))
            T.mm(ps[3][:, 0:N], blk_bf[:], sqk[:, 0:N], True, True, (B_blk, B_sqk), (PS[3],))
            if not is_ctx:
                T.mm(ps[4][:, 0:N], perm_bf[:], ug[:, 0:N], True, True, (B_perm, B_ug), (PS[4],))
            T.act(rstd2[:, 0:N], ps[3][:, 0:N], AF.Sqrt, (PS[3], B_epsb), (B_rstd2,), bias=epsb[:, 0:1], scale=1.0)
            T.recip(rstd2[:, 0:N], rstd2[:, 0:N], (B_rstd2,), (B_rstd2,))
            if is_ctx:
                T.tt(dst, ug[:, 0:N], rstd2[:, 0:N], ALU.mult, (B_ug, B_rstd2), (B_dst,))
            else:
                T.tt(t1[:, 0:N], ug[:, 0:N], ropc[:, 0:N], ALU.mult, (B_ug, B_rope), (B_t1,))
                T.tt(t2[:, 0:N], ps[4][:, 0:N], rops[:, 0:N], ALU.mult, (PS[4], B_rope), (B_t2,))
                T.tt(t1[:, 0:N], t1[:, 0:N], t2[:, 0:N], ALU.add, (B_t1, B_t2), (B_t1,))
                T.tt(dst, t1[:, 0:N], rstd2[:, 0:N], ALU.mult, (B_t1, B_rstd2), (B_dst,))

        for s in range(NSEQ):
            A.reset(m0)
            KT, B_KT = A.t("KT", [128, 4, CL + L], BF16)
            VA, B_VA = A.t("VA", [128, (CL + L) // 128, 8, 65], BF16)
            T.memset(VA[:, :, :, 64:65], 1.0, (B_VA,))
            m1 = A.mark()
            wkv, B_wkv = A.t("wkv", [128, 8, 2048], BF16)
            for k in range(8):
                pass
            T.load("pool", wkv[:], IN["w_in"][l, :, 512:IN_W].rearrange("(k p) f -> p k f", p=128), B_wkv)
            xcs = [A.t(f"xc{i}", [128, 8, 512], F32) for i in range(2)]
            hT, B_hT = A.t("hT", [128, 8, 512], BF16)
            sq = [A.t(f"sq{i}", [128, 512], BF16) for i in range(2)]
            tmp = [A.t(f"tmp{i}", [128, 512], F32) for i in range(2)]
            rstd, B_rstd = A.t("rstd", [128, 512], F32)
            sqk, B_sqk = A.t("sqk", [128, 512], BF16)
            ug, B_ug = A.t("ug", [128, 512], BF16)
            rstd2, B_rstd2 = A.t("rstd2", [128, 512], F32)
            t1, B_t1 = A.t("t1", [128, 512], F32)
            t2, B_t2 = A.t("t2", [128, 512], F32)
            ropc, B_rope = A.t("ropc", [128, 512], F32)
            rops, _ = A.t("rops", [128, 512], F32)
            tC, B_tC = A.t("tC", [128, 512], F32)
            sts = [A.t(f"st{i}", [128, 6, 512], F32) for i in range(2)]
            W = (sqk, B_sqk, ug, B_ug, rstd2, B_rstd2, t1, B_t1, t2, B_t2, ropc, rops, B_rope)

            def load_chunk(ci):
                is_ctx, t0, N, koff = CHUNKS[ci]
                xc, bx = xcs[ci % 2]
                src = cin[s, :, :] if is_ctx else xin[s, :, t0:t0 + N]
                T.load("sp", xc[:, :, 0:N], src.rearrange("(k p) t -> p k t", p=128), bx)

            load_chunk(0)
            for ci, (is_ctx, t0, N, koff) in enumerate(CHUNKS):
                if p1chunks is not None and ci >= p1chunks:
                    break
                if ci + 1 < len(CHUNKS):
                    load_chunk(ci + 1)
                xc, bx = xcs[ci % 2]
                col = 2 if is_ctx else s
                if not is_ctx:
                    T.load("sp", ropc[:, 0:N], IN["ropeC"][:, t0:t0 + N], B_rope)
                    T.load("sp", rops[:, 0:N], IN["ropeS"][:, t0:t0 + N], B_rope)
                rms_modulate(xc[:], bx, N, col, 1, [hT[:, k, 0:N] for k in range(8)], B_hT,
                             [q[0] for q in sq], [q[1] for q in sq], rstd, B_rstd,
                             [q[0] for q in tmp], [q[1] for q in tmp], 0)
                for fc in range(4 if p1cut >= 2 else 0):
                    pb = 1 + fc % 2
                    for k in range(8):
                        T.mm(ps[pb][:, 0:N], wkv[:, k, fc * 128:(fc + 1) * 128], hT[:, k, 0:N], k == 0, k == 7,
                             (B_wkv, B_hT), (PS[pb],))
                    qk_post(pb, N, 1, is_ctx, t0, KT[:, fc, koff:koff + N], B_KT, W)
                for j in range(N // 128 if p1cut >= 3 else 0):
                    pb = 1 + j % 2
                    for k in range(8):
                        T.mm(ps[pb][:, 0:512], hT[:, k, j * 128:(j + 1) * 128], wkv[:, k, 512:1024], k == 0, k == 7,
                             (B_hT, B_wkv), (PS[pb],))
                    kt = koff // 128 + j
                    T.act(VA[:, kt, :, 0:64], ps[pb][:, 0:512].rearrange("p (h d) -> p h d", h=8), AF.Copy,
                          (PS[pb],), (B_VA,))
                st, bst = sts[ci % 2]

                def proj(colbase, c, pb):
                    for k in range(8):
                        T.mm(ps[pb][:, 0:N], wkv[:, k, colbase + c * 128:colbase + (c + 1) * 128], hT[:, k, 0:N],
                             k == 0, k == 7, (B_wkv, B_hT), (PS[pb],))
                if p1cut < 4:
                    continue
                for c in range(2):
                    proj(1024, c, 1)
                    T.act(st[:, c, 0:N], ps[1][:, 0:N], AF.Copy, (PS[1],), (bst,))
                    proj(1280, c, 2)
                    T.act(tC[:, 0:N], ps[2][:, 0:N], AF.Copy, (PS[2],), (B_tC,))
                    proj(1536, c, 1)
                    T.tt(st[:, 2 + c, 0:N], tC[:, 0:N], ps[1][:, 0:N], ALU.mult, (B_tC, PS[1]), (bst,))
                    proj(1792, c, 2)
                    T.act(st[:, 4 + c, 0:N], ps[2][:, 0:N], AF.Copy, (PS[2],), (bst,))
                T.store("sp", stash[s, :, :, koff:koff + N].rearrange("c p t -> p c t"), st[:, :, 0:N], bst)
            dump(f"KT{l}{s}", KT[:], B_KT, [128, 4, CL + L], BF16)
            dump(f"VA{l}{s}", VA[:], B_VA, [128, (CL + L) // 128, 8, 65], BF16)
            T.barrier()
            if stop == "p1":
                break

            A.reset(m1)
            wq, B_wq = A.t("wq", [128, 8, 512], BF16)
            wo, B_wo = A.t("wo", [128, 8, D], BF16)
            T.load("pool", wq[:], IN["w_in"][l, :, 0:512].rearrange("(k p) f -> p k f", p=128), B_wq)
            T.load("pool", wo[:], IN["w_out"][l].rearrange("(k p) f -> p k f", p=128), B_wo)
            xc, B_xc = A.t("xc", [128, 8, 512], F32)
            hT, B_hT = A.t("hT", [128, 8, 512], BF16)
            sq = [A.t(f"sq{i}", [128, 512], BF16) for i in range(2)]
            tmp = [A.t(f"tmp{i}", [128, 512], F32) for i in range(2)]
            rstd, B_rstd = A.t("rstd", [128, 512], F32)
            sqk, B_sqk = A.t("sqk", [128, 512], BF16)
            ug, B_ug = A.t("ug", [128, 512], BF16)
            rstd2, B_rstd2 = A.t("rstd2", [128, 512], F32)
            t1, B_t1 = A.t("t1", [128, 512], F32)
            mt2 = A.mark()
            pt2_alias, _ = A.t("t2", [128, 1024], BF16)
            A.reset(mt2)
            t2, B_t2 = A.t("t2", [128, 512], F32)
            ropc, B_rope = A.t("ropc", [128, 512], F32)
            rops, _ = A.t("rops", [128, 512], F32)
            W = (sqk, B_sqk, ug, B_ug, rstd2, B_rstd2, t1, B_t1, t2, B_t2, ropc, rops, B_rope)
            QT, B_QT = A.t("QT", [128, 4, 512], BF16)
            PT = [A.t(f"PT{i}", [128, 1024], BF16) for i in range(2)]
            PT.append((pt2_alias, B_t2))
            mixed, B_mixed = A.t("mixed", [128, 8, 512], BF16)
            rs, B_rs = A.t("rs", [128, 8], F32)
            o1, B_o1 = A.t("o1", [128, 64], F32)
            ocat, B_ocat = A.t("ocat", [128, 4, 64], F32)
            osq, B_osq = A.t("osq", [128, 4, 64], F32)
            oss, B_oss = A.t("oss", [128, 8], F32)
            onf = [[A.t(f"onf{a}{b}", [128, 128], F32) for b in range(4)] for a in range(2)]
            acnt = [0]
            sB, B_sB = A.t("sB", [128, 2, 512], F32)
            sZ, B_sZ = A.t("sZ", [128, 2, 514], F32)
            sU, B_sU = A.t("sU", [128, 2, 528], F32)
            pa, B_pa = A.t("pa", [128, 528], F32)
            pb_, B_pb = A.t("pb", [128, 528], F32)
            pd, B_pd = A.t("pd", [128, 512], BF16)
            cv, B_cv = t1, B_t1
            h2f, B_h2f = A.t("h2f", [128, 8, 512], F32)
            h2b, B_h2b = hT, B_hT
            lg, B_lg = A.t("lg", [128, NE], F32)
            m8, B_m8 = A.t("m8", [128, 8], F32)
            msk, B_msk = A.t("msk", [128, NE], F32)
            ex, B_ex = A.t("ex", [128, NE], F32)
            rt, B_rt = A.t("rt", [128, 4], F32)
            Gt, B_Gt = A.t("Gt", [128, NE], F32)
            GTc, B_GTc = A.t("GTc", [NE, 512], F32)

            chunks = CHUNKS if not last else CHUNKS[1:]
            if p2chunks is not None:
                chunks = chunks[:p2chunks]
            for (is_ctx, t0, N, koff) in chunks:
                col = 2 if is_ctx else s
                Lq = CL if is_ctx else L
                src = cin[s, :, :] if is_ctx else xin[s, :, t0:t0 + N]
                T.load("sp", xc[:, :, 0:N], src.rearrange("(k p) t -> p k t", p=128), B_xc)
                if not is_ctx:
                    T.load("sp", ropc[:, 0:N], IN["ropeC"][:, t0:t0 + N], B_rope)
                    T.load("sp", rops[:, 0:N], IN["ropeS"][:, t0:t0 + N], B_rope)
                T.load("sp", sB[:, :, 0:N], stash[s, 0:2, :, koff:koff + N].rearrange("c p t -> p c t"), B_sB)
                lo = 1 if t0 == 0 else 0
                hi = 1 if t0 + N == Lq else 0
                if lo:
                    T.memset(sZ[:, :, 0:1], 0.0, (B_sZ,))
                    T.memset(sU[:, :, 0:8], 0.0, (B_sU,))
                if hi:
                    T.memset(sZ[:, :, N + 1:N + 2], 0.0, (B_sZ,))
                    T.memset(sU[:, :, N + 8:N + 16], 0.0, (B_sU,))
                T.load("sp", sZ[:, :, lo:N + 2 - hi],
                       stash[s, 2:4, :, koff - 1 + lo:koff + N + 1 - hi].rearrange("c p t -> p c t"), B_sZ)
                T.load("sp", sU[:, :, 8 * lo:N + 16 - 8 * hi],
                       stash[s, 4:6, :, koff - 8 + 8 * lo:koff + N + 8 - 8 * hi].rearrange("c p t -> p c t"), B_sU)

                rms_modulate(xc[:], B_xc, N, col, 1, [hT[:, k, 0:N] for k in range(8)], B_hT,
                             [q[0] for q in sq], [q[1] for q in sq], rstd, B_rstd,
                             [q[0] for q in tmp], [q[1] for q in tmp], 0)
                for fc in range(4):
                    pbk = 1 + fc % 2
                    for k in range(8):
                        T.mm(ps[pbk][:, 0:N], wq[:, k, fc * 128:(fc + 1) * 128], hT[:, k, 0:N], k == 0, k == 7,
                             (B_wq, B_hT), (PS[pbk],))
                    qk_post(pbk, N, 0, is_ctx, t0, QT[:, fc, 0:N], B_QT, W)

                nk = (CL // 128) if is_ctx else (CL + L) // 128
                nqt = N // 128
                pend = None

                def emit_transposes(hp_, set_):
                    for qt in range(nqt):
                        tb = 4 + qt % 2
                        T.tr(ps[tb][:, 0:128], onf[set_][qt][0][:], ident[:], (onf[set_][qt][1], B_ident), (PS[tb],))
                        T.copy(mixed[:, hp_, qt * 128:(qt + 1) * 128], ps[tb][:, 0:128], (PS[tb],), (B_mixed,))

                for hp in range(4 if p2cut >= 2 else 0):
                    oset = hp % 2
                    for hl in range(2):
                        h = 2 * hp + hl
                        accb = (6, 7)

                        def scores(kt):
                            for c in range(2):
                                bank = 2 * (kt % 3) + c
                                r0 = 64 * hl + 32 * c
                                T.mm(ps[bank][:, 0:N], KT[r0:r0 + 32, hp, kt * 128:(kt + 1) * 128],
                                     QT[r0:r0 + 32, hp, 0:N], True, True, (B_KT, B_QT), (PS[bank],),
                                     tile_position=(r0, 0))
                        scores(0)
                        if nk > 1:
                            scores(1)
                        for kt in range(nk):
                            if kt + 2 < nk:
                                scores(kt + 2)
                            pt, bpt = PT[kt % 3]
                            for c in range(2):
                                bank = 2 * (kt % 3) + c
                                T.act(pt[:, c * 512:c * 512 + N], ps[bank][:, 0:N], AF.Exp, (PS[bank],), (bpt,))
                            for qt in range(nqt):
                                for c in range(2):
                                    bank = accb[qt // 2]
                                    ccol = ((qt % 2) * 2 + c) * 65
                                    T.mm(ps[bank][:, ccol:ccol + 65],
                                         pt[:, c * 512 + qt * 128:c * 512 + qt * 128 + 128], VA[:, kt, h, :],
                                         kt == 0 and qt % 2 == 0 and c == 0, kt == nk - 1, (bpt, B_VA), (PS[bank],),
                                         skip_group_check=True)
                        for qt in range(nqt):
                            bank = accb[qt // 2]
                            base = (qt % 2) * 130
                            acc = ps[bank][:, base:base + 130].rearrange("p (c d) -> p c d", c=2)
                            BA = PS[bank]
                            T.recip(rs[:, 0:2], acc[:, :, 64], (BA,), (B_rs,))
                            T.ts(rs[:, 2:3], rs[:, 1:2], nlam, None, ALU.mult, (B_rs, B_lamt), (B_rs,))
                            T.ts(o1[:], acc[:, 0, 0:64], rs[:, 0:1], None, ALU.mult, (BA, B_rs), (B_o1,))
                            T.stt(ocat[:, qt, :], acc[:, 1, 0:64], rs[:, 2:3], o1[:], ALU.mult, ALU.add,
                                  (BA, B_rs, B_o1), (B_ocat,))
                        T.tt(osq[:, 0:nqt, :], ocat[:, 0:nqt, :], ocat[:, 0:nqt, :], ALU.mult, (B_ocat,), (B_osq,))
                        T.rsum(oss[:, 0:nqt], osq[:, 0:nqt, :], (B_osq,), (B_oss,))
                        T.act(oss[:, 4:4 + nqt], oss[:, 0:nqt], AF.Ln, (B_oss, B_epsb), (B_oss,), bias=epsb[:, 1:2], scale=1.0)
                        T.act(oss[:, 4:4 + nqt], oss[:, 4:4 + nqt], AF.Exp, (B_oss,), (B_oss,), scale=-0.5)
                        for qt in range(nqt):
                            of, bof = onf[oset][qt]
                            T.stt(of[:, hl * 64:(hl + 1) * 64], ocat[:, qt, :], oss[:, 4 + qt:5 + qt],
                                  subgb[:, hl * 64:(hl + 1) * 64], ALU.mult, ALU.mult, (B_ocat, B_oss, B_subg), (bof,))
                        if hl == 0 and pend is not None:
                            emit_transposes(*pend)
                            pend = None
                    pend = (hp, oset)
                if pend is not None:
                    emit_transposes(*pend)

                if p2cut < 4:
                    continue
                for c in range(2):
                    T.ts(cv[:, 0:N], sZ[:, c, 0:N], convw[:, c, 0:1], None, ALU.mult, (B_sZ, B_convw), (B_cv,))
                    T.stt(cv[:, 0:N], sZ[:, c, 1:N + 1], convw[:, c, 1:2], cv[:, 0:N], ALU.mult, ALU.add,
                          (B_sZ, B_convw, B_cv), (B_cv,))
                    T.stt(cv[:, 0:N], sZ[:, c, 2:N + 2], convw[:, c, 2:3], cv[:, 0:N], ALU.mult, ALU.add,
                          (B_sZ, B_convw, B_cv), (B_cv,))
                    T.tt(mixed[:, 4 + c, 0:N], cv[:, 0:N], sB[:, c, 0:N], ALU.mult, (B_cv, B_sB), (B_mixed,))
                M = N + 16
                for c in range(2):
                    u = sU[:, c, :]
                    T.tt(pa[:, 1:M], u[:, 0:M - 1], u[:, 1:M], ALU.add, (B_sU,), (B_pa,))
                    if c == 0:
                        T.tt(pb_[64:128, 2:M - 1], pa[64:128, 1:M - 2], pa[64:128, 3:M], ALU.add, (B_pa,), (B_pb,))
                        T.copy(pb_[0:64, 8:8 + N], pa[0:64, 8:8 + N], (B_pa,), (B_pb,))
                        fin = pb_
                        B_fin = B_pb
                    else:
                        T.tt(pb_[:, 2:M - 1], pa[:, 1:M - 2], pa[:, 3:M], ALU.add, (B_pa,), (B_pb,))
                        T.tt(pa[:, 4:M - 3], pb_[:, 2:M - 5], pb_[:, 6:M - 1], ALU.add, (B_pb,), (B_pa,))
                        T.tt(pb_[64:128, 8:M - 7], pa[64:128, 4:M - 11], pa[64:128, 12:M - 3], ALU.add, (B_pa,), (B_pb,))
                        T.copy(pb_[0:64, 8:8 + N], pa[0:64, 8:8 + N], (B_pa,), (B_pb,))
                        fin = pb_
                        B_fin = B_pb
                    T.ts(pa[:, 0:N], fin[:, 8:8 + N], pwinv[:, c:c + 1], None, ALU.mult, (B_fin, B_pwinv), (B_pa,))
                    if lo:
                        T.tt(pa[:, 0:8], fin[:, 8:16], pedge[:, c, 0:8], ALU.mult, (B_fin, B_pedge), (B_pa,))
                    if hi:
                        T.tt(pa[:, N - 8:N], fin[:, N:N + 8], pedge[:, c, 8:16], ALU.mult, (B_fin, B_pedge), (B_pa,))
                    T.tt(pd[:, 0:N], pa[:, 0:N], u[:, 8:8 + N], ALU.subtract, (B_pa, B_sU), (B_pd,))
                    T.mm(ps[7][:, 0:N], poolbd[:, c, :], pd[:, 0:N], True, True, (B_poolbd, B_pd), (PS[7],))
                    T.ts(mixed[:, 6 + c, 0:N], ps[7][:, 0:N], pools[:, c:c + 1], None, ALU.mult, (PS[7], B_pools), (B_mixed,))
                if p2cut < 5:
                    continue
                for dc in range(8):
                    pbk = 6 + dc % 2
                    for k in range(8):
                        T.mm(ps[pbk][:, 0:N], wo[:, k, dc * 128:(dc + 1) * 128], mixed[:, k, 0:N], k == 0, k == 7,
                             (B_wo, B_mixed), (PS[pbk],))
                    T.stt(xc[:, dc, 0:N], ps[pbk][:, 0:N], modT[:, 16 + dc, col:col + 1], xc[:, dc, 0:N],
                          ALU.mult, ALU.add, (PS[pbk], B_mod, B_xc), (B_xc,))
                dst = c1res[s, :, :] if is_ctx else x1res[s, :, t0:t0 + N]
                T.store("sp", dst.rearrange("(k p) t -> p k t", p=128), xc[:, :, 0:N], B_xc)
                if p2cut < 6:
                    continue
                rms_modulate(xc[:], B_xc, N, col, 2, [h2f[:, k, 0:N] for k in range(8)], B_h2f,
                             [q[0] for q in sq], [q[1] for q in sq], rstd, B_rstd,
                             [q[0] for q in tmp], [q[1] for q in tmp], 0)
                T.copy(h2b[:, :, 0:N], h2f[:, :, 0:N], (B_h2f,), (B_h2b,))
                tok0 = (s * L + t0) if not is_ctx else (NSEQ * L + s * CL)
                T.store("sp", h2T[:, tok0:tok0 + N].rearrange("(k p) t -> p k t", p=128), h2b[:, :, 0:N], B_h2b)
                for j in range(N // 128):
                    for k in range(8):
                        T.mm(ps[1][:, 0:NE], h2f[:, k, j * 128:(j + 1) * 128], wr[:, k, :], k == 0, k == 7,
                             (B_h2f, B_wr), (PS[1],))
                    T.tt(lg[:], ps[1][:, 0:NE], rbb[:], ALU.add, (PS[1], B_rbb), (B_lg,))
                    T.max8(m8[:], lg[:], (B_lg,), (B_m8,))
                    T.ts(msk[:], lg[:], m8[:, 3:4], None, ALU.is_ge, (B_lg, B_m8), (B_msk,))
                    T.ts(rt[:, 0:1], m8[:, 0:1], -1.0, None, ALU.mult, (B_m8,), (B_rt,))
                    T.act(ex[:], lg[:], AF.Exp, (B_lg, B_rt), (B_ex,), bias=rt[:, 0:1], scale=1.0)
                    T.tt(ex[:], ex[:], msk[:], ALU.mult, (B_ex, B_msk), (B_ex,))
                    T.rsum(rt[:, 1:2], ex[:], (B_ex,), (B_rt,))
                    T.recip(rt[:, 2:3], rt[:, 1:2], (B_rt,), (B_rt,))
                    T.ts(Gt[:], ex[:], rt[:, 2:3], None, ALU.mult, (B_ex, B_rt), (B_Gt,))
                    T.tr(ps[2][0:NE, 0:128], Gt[:], ident[:], (B_Gt, B_ident), (PS[2],))
                    T.act(GTc[:, j * 128:(j + 1) * 128], ps[2][0:NE, 0:128], AF.Copy, (PS[2],), (B_GTc,))
                T.store("sp", GT[:, tok0:tok0 + N], GTc[:, 0:N], B_GTc)
            T.barrier()
            if stop == "p2":
                break
        if stop in ("p1", "p2", "p2all"):
            break

        A.reset(m0)
        Ttok = NSEQ * L + (0 if last else NSEQ * CL)
        wgu = [A.t(f"wgu{i}", [128, 8, 2 * D], BF16) for i in range(2)]
        wdn = [A.t(f"wdn{i}", [128, 8, D], BF16) for i in range(2)]
        mh = A.mark()
        x1c, B_x1c = A.t("h2g", [128, 8, 512], F32)
        A.reset(mh)
        h2g, B_h2g = A.t("h2g", [128, 8, TG], BF16)
        assert B_x1c is B_h2g
        GTg, B_GTg = A.t("GTg", [NE, TG], F32)
        yacc, B_yacc = A.t("yacc", [128, 8, TG], F32)
        actT = [A.t(f"actT{i}", [128, 8, 512], BF16) for i in range(2)]
        gbc = [A.t(f"gbc{i}", [128, 512], F32) for i in range(2)]
        g1s = [A.t(f"g1_{i}", [128, 512], F32) for i in range(2)]
        sgs = [A.t(f"sg_{i}", [128, 512], F32) for i in range(2)]
        l1s = [A.t(f"l1_{i}", [128, 512], F32) for i in range(2)]
        ngroups = (Ttok + TG - 1) // TG
        itc = [0]
        for g in range(ngroups):
            g0 = g * TG
            gn = min(TG, Ttok - g0)
            ntc = gn // 512
            T.load("sp", h2g[:, :, 0:gn], h2T[:, g0:g0 + gn].rearrange("(k p) t -> p k t", p=128), B_h2g)
            T.load("sp", GTg[:, 0:gn], GT[:, g0:g0 + gn], B_GTg)
            for tc in range(ntc):
                for dc in range(8):
                    pbk = 4 + dc % 2
                    T.mm(ps[pbk][:, :], bdn[:, dc * 128:(dc + 1) * 128], GTg[:, tc * 512:(tc + 1) * 512], True, True,
                         (B_bdn, B_GTg), (PS[pbk],))
                    T.act(yacc[:, dc, tc * 512:(tc + 1) * 512], ps[pbk][:, :], AF.Copy, (PS[pbk],), (B_yacc,))
            units = [(e_, tc) for e_ in range(ne_run) for tc in range(ntc)]
            ustate = {}

            def unit_setup(u):
                e_, tc = u
                if tc == 0:
                    wg, bwg = wgu[e_ % 2]
                    wd, bwd = wdn[e_ % 2]
                    T.load("pool", wg[:], IN["w_gate_up"][l, e_].rearrange("(k p) f -> p k f", p=128), bwg)
                    T.load("pool", wd[:], IN["w_down"][l, e_].rearrange("(k p) f -> p k f", p=128), bwd)
                gb, bgb = gbc[itc[0] % 2]
                at, bat = actT[itc[0] % 2]
                itc[0] += 1
                T.load("sp", gb[:], GT[e_, g0 + tc * 512:g0 + (tc + 1) * 512].partition_broadcast(128), bgb)
                ustate[u] = (gb, bgb, at, bat)

            def emit_gl(u, fcs):
                e_, tc = u
                if u not in ustate:
                    unit_setup(u)
                gb, bgb, at, bat = ustate[u]
                wg, bwg = wgu[e_ % 2]
                rhs_t = slice(tc * 512, (tc + 1) * 512)
                for fc in fcs:
                    pA = fc % 2
                    pB = 2 + fc % 2
                    g1, B_g1 = g1s[fc % 2]
                    sg, B_sg = sgs[fc % 2]
                    l1, B_l1 = l1s[fc % 2]
                    for k in range(8):
                        T.mm(ps[pA][:, :], wg[:, k, fc * 128:(fc + 1) * 128], h2g[:, k, rhs_t], k == 0, k == 7,
                             (bwg, B_h2g), (PS[pA],))
                    for k in range(8):
                        T.mm(ps[pB][:, :], wg[:, k, D + fc * 128:D + (fc + 1) * 128], h2g[:, k, rhs_t], k == 0, k == 7,
                             (bwg, B_h2g), (PS[pB],))
                    T.ts(g1[:], ps[pA][:, :], bgu[:, e_, fc:fc + 1], 7.0, ALU.add, (PS[pA], B_bgu), (B_g1,), op1=ALU.min)
                    T.act(sg[:], g1[:], AF.Sigmoid, (B_g1,), (B_sg,), scale=1.702)
                    T.ts(l1[:], ps[pB][:, :], bgu[:, e_, 8 + fc:9 + fc], 8.0, ALU.add, (PS[pB], B_bgu), (B_l1,), op1=ALU.min)
                    T.stt(l1[:], l1[:], -6.0, gb[:], ALU.max, ALU.mult, (B_l1, bgb), (B_l1,))
                    T.tt(g1[:], g1[:], sg[:], ALU.mult, (B_g1, B_sg), (B_g1,))
                    T.tt(at[:, fc, :], g1[:], l1[:], ALU.mult, (B_g1, B_l1), (bat,))

            def emit_down(u):
                e_, tc = u
                gb, bgb, at, bat = ustate.pop(u)
                wd, bwd = wdn[e_ % 2]
                rhs_t = slice(tc * 512, (tc + 1) * 512)
                for dc in range(8):
                    pY = 4 + dc % 2
                    for k in range(8):
                        T.mm(ps[pY][:, :], wd[:, k, dc * 128:(dc + 1) * 128], at[:, k, :], k == 0, k == 7,
                             (bwd, bat), (PS[pY],))
                    T.tt(yacc[:, dc, rhs_t], yacc[:, dc, rhs_t], ps[pY][:, :], ALU.add, (B_yacc, PS[pY]), (B_yacc,))

            if units:
                emit_gl(units[0], range(8))
            for ui, u in enumerate(units):
                nxt = units[ui + 1] if ui + 1 < len(units) else None
                if nxt is not None:
                    emit_gl(nxt, range(0, 2))
                emit_down(u)
                if nxt is not None:
                    emit_gl(nxt, range(2, 8))
            for tc in range(ntc):
                tok = g0 + tc * 512
                if tok < NSEQ * L:
                    s_ = tok // L
                    t0 = tok % L
                    col = s_
                    srcs = [(x1res[s_, :, t0:t0 + 512], xout[s_, :, t0:t0 + 512], 0, 512)]
                else:
                    col = 2
                    srcs = [(c1res[s_, :, :], c2res[s_, :, :], s_ * CL, CL) for s_ in range(NSEQ)]
                for (sap, dap, o, n) in srcs:
                    T.load("sp", x1c[:, :, o:o + n], sap.rearrange("(k p) t -> p k t", p=128), B_x1c)
                for dc in range(8):
                    T.stt(x1c[:, dc, :], yacc[:, dc, tc * 512:(tc + 1) * 512], modT[:, 40 + dc, col:col + 1], x1c[:, dc, :],
                          ALU.mult, ALU.add, (B_yacc, B_mod, B_x1c), (B_x1c,))
                for (sap, dap, o, n) in srcs:
                    T.store("sp", dap.rearrange("(k p) t -> p k t", p=128), x1c[:, :, o:o + n], B_x1c)
        T.barrier()

    if depth == DEPTH and stop_ is None:
        B_fin = Buf("fincopy")
        for s in range(NSEQ):
            T.dma("sp", out[s], x2res[s], B_fin)
    T.emit(nc)
    nc._trk = T
    nc._in_names = list(IN.keys())
    nc._alloc_peak = A.peak
    return nc


_CACHE = {}


def kernel(**inputs):
    I = {k: np.asarray(v) for k, v in inputs.items()}
    S = _shared_inputs(I)
    if "nc" not in _CACHE:
        _CACHE["nc"] = build()
    nc = _CACHE["nc"]
    in_maps = []
    for core in range(NCORES):
        m = dict(S)
        m.update(_core_inputs(I, core))
        in_maps.append({k: m[k] for k in nc._in_names})
    res = run_bass_kernel_spmd(nc, in_maps, core_ids=list(range(NCORES)))
    outs = [np.transpose(np.asarray(r["outT"]), (0, 2, 1)) for r in res.results]
    return np.ascontiguousarray(np.concatenate(outs, axis=0), dtype=np.float32)
```

```python
import math
import numpy as np
import ml_dtypes
import concourse.bass as bass
import concourse.mybir as mybir
from concourse.bass_utils import run_bass_kernel_spmd

F32 = mybir.dt.float32
BF16 = mybir.dt.bfloat16
AF = mybir.ActivationFunctionType
ALU = mybir.AluOpType
AX = mybir.AxisListType

D = 1024
L = 4096
CL = 256
NCORES = 8
NSEQ = 2
DEPTH = 2
IN_W = 2560
NE = 32
EPS = 1e-6
TG = 1024


class Buf:
    __slots__ = ("name", "last_w", "readers", "sem", "cnt", "excl")

    def __init__(self, name, excl=False):
        self.name = name
        self.excl = excl
        self.last_w = None
        self.readers = {}
        self.sem = None
        self.cnt = 0


class Ins:
    __slots__ = ("eng", "fn", "deps", "need_inc", "ev", "dma_buf", "snap")

    def __init__(self, eng, fn, dma_buf=None):
        self.eng = eng
        self.fn = fn
        self.deps = []
        self.need_inc = False
        self.ev = None
        self.dma_buf = dma_buf
        self.snap = None


class Tracker:
    ENGS = ("pe", "act", "dve", "pool", "sp")

    def __init__(self):
        self.prog = []
        self.last_on = {e: None for e in self.ENGS}
        self.dma_bufs = []

    def add(self, eng, fn, r=(), w=(), dma_buf=None):
        ins = Ins(eng, fn, dma_buf)
        if any(b.excl for b in r):
            w = tuple(w) + tuple(b for b in r if b.excl)
            r = tuple(b for b in r if not b.excl)
        deps = {}
        for b in r:
            if b.last_w is not None:
                deps[id(b.last_w)] = b.last_w
        for b in w:
            if b.last_w is not None:
                deps[id(b.last_w)] = b.last_w
            for rd in b.readers.values():
                deps[id(rd)] = rd
        deps.pop(id(ins), None)
        for d in deps.values():
            if d.eng == "pe" and eng == "pe" and d.dma_buf is None and dma_buf is None:
                continue
            ins.deps.append(d)
            d.need_inc = True
        key = eng if dma_buf is None else ("dma", id(dma_buf))
        for b in r:
            b.readers[key] = ins
        for b in w:
            b.last_w = ins
            b.readers = {}
        if dma_buf is not None and dma_buf.sem is None:
            dma_buf.sem = "pending"
            self.dma_bufs.append(dma_buf)
        self.prog.append(ins)
        if dma_buf is None:
            self.last_on[eng] = ins
        return ins

    def barrier(self):
        for e in self.ENGS:
            li = self.last_on[e]
            if li is not None:
                li.need_inc = True
        self.prog.append("barrier")

    def mm(self, out, lhsT, rhs, start, stop, r, w, **kw):
        return self.add("pe", lambda e: e.matmul(out, lhsT, rhs, start=start, stop=stop, **kw), r, w)

    def tr(self, out, in_, ident, r, w):
        return self.add("pe", lambda e: e.transpose(out, in_, ident), r, w)

    def act(self, out, in_, func, r, w, **kw):
        return self.add("act", lambda e: e.activation(out=out, in_=in_, func=func, **kw), r, w)

    def ts(self, out, in0, s1, s2, op0, r, w, op1=None, eng="dve"):
        if op1 is None:
            return self.add(eng, lambda e: e.tensor_scalar(out, in0, s1, None, op0), r, w)
        return self.add(eng, lambda e: e.tensor_scalar(out, in0, s1, s2, op0, op1), r, w)

    def tt(self, out, in0, in1, op, r, w, eng="dve"):
        return self.add(eng, lambda e: e.tensor_tensor(out, in0, in1, op), r, w)

    def stt(self, out, in0, scalar, in1, op0, op1, r, w, eng="dve", **kw):
        return self.add(eng, lambda e: e.scalar_tensor_tensor(out, in0, scalar, in1, op0, op1, **kw), r, w)

    def copy(self, out, in_, r, w, eng="dve"):
        return self.add(eng, lambda e: e.tensor_copy(out, in_), r, w)

    def recip(self, out, in_, r, w):
        return self.add("dve", lambda e: e.reciprocal(out, in_), r, w)

    def rsum(self, out, in_, r, w):
        return self.add("dve", lambda e: e.reduce_sum(out, in_, AX.X), r, w)

    def max8(self, out, in_, r, w):
        return self.add("dve", lambda e: e.max(out=out, in_=in_), r, w)

    def memset(self, ap, val, w, eng="dve"):
        return self.add(eng, lambda e: e.memset(ap, val), (), w)

    def dma(self, eng, out, in_, buf, r=(), w=()):
        return self.add(eng, lambda e: e.dma_start(out=out, in_=in_), r, w, dma_buf=buf)

    def load(self, eng, out, in_, buf):
        return self.dma(eng, out, in_, buf, r=(), w=(buf,))

    def store(self, eng, out, in_, buf):
        return self.dma(eng, out, in_, buf, r=(buf,), w=())

    def emit(self, nc):
        engsem = {e: nc.alloc_semaphore("sem_" + e) for e in self.ENGS}
        cnt = {e: 0 for e in self.ENGS}
        for b in self.dma_bufs:
            b.sem = nc.alloc_semaphore("dsem_" + b.name)
        lists = {e: [] for e in self.ENGS}
        for ins in self.prog:
            if ins == "barrier":
                snap = [(engsem[e], cnt[e]) for e in self.ENGS if cnt[e] > 0]
                snap += [(b.sem, b.cnt) for b in self.dma_bufs if b.cnt > 0]
                for e in self.ENGS:
                    lists[e].append(("barrier", snap))
                continue
            if ins.dma_buf is not None:
                b = ins.dma_buf
                b.cnt += 16
                ins.ev = (b.sem, b.cnt)
            elif ins.need_inc:
                cnt[ins.eng] += 1
                ins.ev = (engsem[ins.eng], cnt[ins.eng])
            lists[ins.eng].append(ins)
        final = [(engsem[e], cnt[e]) for e in self.ENGS if cnt[e] > 0]
        final += [(b.sem, b.cnt) for b in self.dma_bufs if b.cnt > 0]
        self.n_ins = {e: len(lists[e]) for e in self.ENGS}

        def replay(e, name):
            waited = {}

            def wait(sem, val):
                k = id(sem)
                if waited.get(k, 0) < val:
                    e.wait_ge(sem, val)
                    waited[k] = val

            for ins in lists[name]:
                if isinstance(ins, tuple):
                    for sem, val in ins[1]:
                        wait(sem, val)
                    continue
                for d in ins.deps:
                    wait(*d.ev)
                bi = ins.fn(e)
                if ins.dma_buf is not None:
                    bi.then_inc(ins.ev[0], 16)
                elif ins.need_inc:
                    bi.then_inc(ins.ev[0], 1)
            if name == "sp":
                for sem, val in final:
                    wait(sem, val)

        with nc.Block() as block:
            @block.tensor
            def _(e):
                replay(e, "pe")

            @block.scalar
            def _(e):
                replay(e, "act")

            @block.vector
            def _(e):
                replay(e, "dve")

            @block.gpsimd
            def _(e):
                replay(e, "pool")

            @block.sync
            def _(e):
                replay(e, "sp")


class Alloc:
    def __init__(self, nc, limit=229376 - 256):
        self.nc = nc
        self.off = 16640
        self.limit = limit
        self.n = 0
        self.peak = 0
        self.bufs = {}
        self.handles = {}

    def t(self, name, shape, dtype):
        size = 1
        for s in shape[1:]:
            size *= s
        size *= 2 if dtype == BF16 else 4
        off = (self.off + 63) // 64 * 64
        assert off + size <= self.limit, f"SBUF overflow at {name}: {off + size}"
        key = (name, off, tuple(shape), str(dtype))
        if key in self.handles:
            h = self.handles[key]
        else:
            self.n += 1
            h = self.nc.alloc_sbuf_tensor_at(f"{name}_{self.n}", list(shape), dtype, offset=off)
            self.handles[key] = h
        self.off = off + size
        self.peak = max(self.peak, self.off)
        if name not in self.bufs:
            self.bufs[name] = Buf(name)
        return h, self.bufs[name]

    def mark(self):
        return self.off

    def reset(self, m):
        self.off = m


def _consts():
    p = np.arange(128)
    i = p % 32
    axis = i // 16
    half = (i % 16) // 8
    pair = i % 8
    t = np.arange(L)
    row = (t // 64).astype(np.float32)
    col = (t % 64).astype(np.float32)
    inv = (np.float32(10000.0) ** (-np.arange(8, dtype=np.float32) / np.float32(8))).astype(np.float32)
    pos = np.where(axis[:, None] == 0, row[None, :], col[None, :]).astype(np.float32)
    ang = (pos * inv[pair][:, None]).astype(np.float32)
    ropeC = np.cos(ang).astype(np.float32)
    ropeS = (np.sin(ang) * np.where(half == 0, -1.0, 1.0)[:, None]).astype(np.float32)
    partner = np.where(half == 0, p + 8, p - 8)
    perm = np.zeros((128, 128), np.float32)
    perm[partner, p] = 1.0
    blk = (p[:, None] // 32 == p[None, :] // 32).astype(np.float32) / 32.0
    ident = np.eye(128, dtype=np.float32)
    wins = (2, 4, 8, 16)
    pwinv = np.zeros((128, 2), np.float32)
    pedge = np.zeros((128, 2, 16), np.float32)
    for c in range(2):
        for gl in range(2):
            w = wins[2 * c + gl]
            hf = w // 2
            sl = slice(gl * 64, gl * 64 + 64)
            pwinv[sl, c] = 1.0 / w
            for j in range(8):
                pedge[sl, c, j] = 1.0 / (min(j + hf, 1 << 30) - max(j - hf, 0))
                dist = 8 - j
                pedge[sl, c, 8 + j] = 1.0 / (min(hf, dist) + hf)
    sel = np.zeros((32, 32, 128), np.float32)
    return dict(ropeC=ropeC, ropeS=ropeS, perm=perm, blk=blk, ident=ident, pwinv=pwinv, pedge=pedge)


def _fm(v):
    sh = v.shape
    n = sh[-1] // 128
    return np.ascontiguousarray(np.swapaxes(v.reshape(*sh[:-1], n, 128), -1, -2))


def _shared_inputs(I):
    f = np.float32
    S = {}
    S["w_mod"] = np.ascontiguousarray(I["w_mod"], dtype=f)
    S["bmodx"] = np.ascontiguousarray(np.repeat(_fm(I["b_mod"])[..., None], 3, axis=-1), dtype=f)
    S["n1gx"] = np.ascontiguousarray(np.repeat(_fm(I["norm1_g"])[..., None], 3, axis=-1), dtype=f)
    S["n2gx"] = np.ascontiguousarray(np.repeat(_fm(I["norm2_g"])[..., None], 3, axis=-1), dtype=f)
    S["w_in"] = np.ascontiguousarray(I["w_in"], dtype=f)
    S["w_out"] = np.ascontiguousarray(I["w_out"], dtype=f)
    qk = np.stack([np.tile(I["q_norm_g"], (1, 4)), np.tile(I["k_norm_g"], (1, 4))], axis=-1)
    S["qkg"] = np.ascontiguousarray(qk, dtype=f)
    S["lam4"] = np.ascontiguousarray(np.stack([I["lambda_q1"], I["lambda_k1"], I["lambda_q2"], I["lambda_k2"]], axis=1), dtype=f)
    S["subg2"] = np.ascontiguousarray(np.tile(I["subln_g"], (1, 2)), dtype=f)
    cw = I["conv_w"]
    S["convw"] = np.ascontiguousarray(np.transpose(cw.reshape(2, 3, 2, 128), (0, 3, 2, 1)), dtype=f)
    pw = I["pool_w"]
    bd = np.zeros((2, 2, 128, 128), f)
    for l in range(2):
        for c in range(2):
            for gl in range(2):
                bd[l, c, gl * 64:(gl + 1) * 64, gl * 64:(gl + 1) * 64] = pw[l, 2 * c + gl]
    S["poolbd"] = bd
    S["pools"] = np.ascontiguousarray(_fm(I["pool_scale"]), dtype=f)
    S["router_w"] = np.ascontiguousarray(I["router_w"], dtype=f)
    S["router_b"] = np.ascontiguousarray(I["router_b"], dtype=f)
    S["w_gate_up"] = np.ascontiguousarray(I["w_gate_up"], dtype=f)
    S["w_down"] = np.ascontiguousarray(I["w_down"], dtype=f)
    S["bguT"] = np.ascontiguousarray(np.transpose(_fm(I["b_gate_up"]), (0, 2, 1, 3)), dtype=f)
    S["b_down"] = np.ascontiguousarray(I["b_down"], dtype=f)
    S.update(_consts())
    return S


def _core_inputs(I, core):
    b0 = NSEQ * core
    f = np.float32
    C = {}
    C["xT"] = np.ascontiguousarray(np.transpose(I["x"][b0:b0 + NSEQ], (0, 2, 1)), dtype=f)
    C["ctxT"] = np.ascontiguousarray(np.transpose(I["ctx"][b0:b0 + NSEQ], (0, 2, 1)), dtype=f)
    cc = np.stack([I["c"][b0], I["c"][b0 + 1], I["c_ctx"]], axis=-1)
    C["ccT"] = np.ascontiguousarray(np.transpose(cc.reshape(8, 128, 3), (1, 0, 2)), dtype=f)
    return C


IN_SHAPES = dict(
    xT=[NSEQ, D, L], ctxT=[NSEQ, D, CL], ccT=[128, 8, 3],
    w_mod=[2, D, 6 * D], bmodx=[2, 128, 48, 3], n1gx=[2, 128, 8, 3], n2gx=[2, 128, 8, 3],
    w_in=[2, D, IN_W], w_out=[2, D, D], qkg=[2, 128, 2], lam4=[2, 4, 32], subg2=[2, 128],
    convw=[2, 128, 2, 3], poolbd=[2, 2, 128, 128], pools=[2, 128, 2],
    router_w=[2, D, NE], router_b=[2, NE], w_gate_up=[2, NE, D, 2 * D], w_down=[2, NE, D, D],
    bguT=[2, 128, NE, 16], b_down=[2, NE, D],
    ropeC=[128, L], ropeS=[128, L], perm=[128, 128], blk=[128, 128], ident=[128, 128],
    pwinv=[128, 2], pedge=[128, 2, 16],
)


def build(depth=DEPTH, dbg=False, stop=None, rowtile=True, p1cut=4, p1chunks=None, p2cut=6, p2chunks=None, ne_run=NE, stop_layer=0):
    nc = bass.Bass("TRN2", target_bir_lowering=False)
    T = Tracker()
    A = Alloc(nc)
    class _LazyIn(dict):
        def __missing__(self, k):
            v = nc.dram_tensor(k, IN_SHAPES[k], F32, kind="ExternalInput").ap()
            self[k] = v
            return v

    IN = _LazyIn()
    out = nc.dram_tensor("outT", [NSEQ, D, L], F32, kind="ExternalOutput").ap()
    skind = "ExternalOutput" if dbg else "Internal"

    def scratch(name, shape, dt=F32):
        return nc.dram_tensor(name, shape, dt, kind=skind).ap()

    x1res = scratch("x1res", [NSEQ, D, L])
    x2res = scratch("x2res", [NSEQ, D, L])
    c1res = scratch("c1res", [NSEQ, D, CL])
    c2res = scratch("c2res", [NSEQ, D, CL])
    stash = scratch("stash", [NSEQ, 6, 128, CL + L])
    TT = NSEQ * L + NSEQ * CL
    h2T = scratch("h2T", [D, TT], BF16)
    GT = scratch("GT", [NE, TT])

    def dump(name, ap, buf, shape, dt=F32):
        if dbg:
            d_ = nc.dram_tensor("dbg_" + name, list(shape), dt, kind="ExternalOutput").ap()
            T.store("sp", d_, ap, buf)

    ps = [nc.alloc_psum_tensor(f"ps{i}", [128, 512], F32) for i in range(8)]
    PS = [Buf(f"ps{i}", excl=True) for i in range(8)]

    ones_bf, B_ones = A.t("ones", [128, 128], BF16)
    blk_bf, B_blk = A.t("blk", [128, 128], BF16)
    perm_bf, B_perm = A.t("perm", [128, 128], BF16)
    ident, B_ident = A.t("ident", [128, 128], F32)
    sc, B_sc = A.t("sc", [128, 8, 3], F32)
    modT, B_mod = A.t("modT", [128, 48, 3], F32)
    gm, B_gm = A.t("gm", [128, 16, 3], F32)
    bmodx, B_bmodx = A.t("bmodx", [128, 48, 3], F32)
    ngx, B_ngx = A.t("ngx", [128, 16, 3], F32)
    qkg, B_qkg = A.t("qkg", [128, 2], F32)
    qkgs, B_qkgs = A.t("qkgs", [128, 2], F32)
    lam4b, B_lam4 = A.t("lam4b", [128, 4, 32], F32)
    lamt, B_lamt = A.t("lamt", [128, 8], F32)
    subgb, B_subg = A.t("subgb", [128, 128], F32)
    convw, B_convw = A.t("convw", [128, 2, 3], F32)
    pools, B_pools = A.t("pools", [128, 2], F32)
    pwinv, B_pwinv = A.t("pwinv", [128, 2], F32)
    pedge, B_pedge = A.t("pedge", [128, 2, 16], F32)
    poolbd, B_poolbd = A.t("poolbd", [128, 2, 128], BF16)
    wr, B_wr = A.t("wr", [128, 8, NE], F32)
    rbb, B_rbb = A.t("rbb", [128, NE], F32)
    bgu, B_bgu = A.t("bgu", [128, NE, 16], F32)
    bdn, B_bdn = A.t("bdn", [NE, D], F32)

    epsb, B_epsb = A.t("epsb", [128, 2], F32)
    T.memset(epsb[:, 0:1], EPS, (B_epsb,))
    T.memset(epsb[:, 1:2], 64.0 * EPS, (B_epsb,))
    T.memset(ones_bf[:], 1.0 / 1024.0, (B_ones,))
    T.load("pool", blk_bf[:], IN["blk"], B_blk)
    T.load("pool", perm_bf[:], IN["perm"], B_perm)
    T.load("sp", ident[:], IN["ident"], B_ident)
    T.load("sp", sc[:], IN["ccT"], B_sc)
    T.load("sp", pwinv[:], IN["pwinv"], B_pwinv)
    T.load("sp", pedge[:], IN["pedge"], B_pedge)
    T.act(sc[:], sc[:], AF.Silu, (B_sc,), (B_sc,))
    base_mark = A.mark()

    CHUNKS = [(True, 0, CL, 0)] + [(False, 512 * i, 512, CL + 512 * i) for i in range(L // 512)]

    stop_ = stop
    for l in range(depth):
        stop = stop_ if l == stop_layer else None
        last = (l == DEPTH - 1)
        lam_init = 0.8 - 0.6 * math.exp(-0.3 * l)
        xin = IN["xT"] if l == 0 else x2res
        cin = IN["ctxT"] if l == 0 else c2res
        xout = x2res

        A.reset(base_mark)
        T.load("sp", bmodx[:], IN["bmodx"][l], B_bmodx)
        T.load("sp", ngx[:, 0:8, :], IN["n1gx"][l], B_ngx)
        T.load("sp", ngx[:, 8:16, :], IN["n2gx"][l], B_ngx)
        T.load("sp", qkg[:], IN["qkg"][l], B_qkg)
        T.load("sp", lam4b[:], IN["lam4"][l].partition_broadcast(128), B_lam4)
        T.load("sp", subgb[:], IN["subg2"][l].partition_broadcast(128), B_subg)
        T.load("sp", convw[:], IN["convw"][l], B_convw)
        T.load("sp", pools[:], IN["pools"][l], B_pools)
        T.load("pool", poolbd[:], IN["poolbd"][l].rearrange("c p j -> p c j"), B_poolbd)
        T.load("sp", wr[:], IN["router_w"][l].rearrange("(k p) e -> p k e", p=128), B_wr)
        T.load("sp", rbb[:], IN["router_b"][l].partition_broadcast(128), B_rbb)
        T.load("sp", bgu[:], IN["bguT"][l], B_bgu)
        T.load("sp", bdn[:], IN["b_down"][l], B_bdn)
        T.ts(bgu[:, :, 8:16], bgu[:, :, 8:16], 1.0, None, ALU.add, (B_bgu,), (B_bgu,))
        T.ts(qkgs[:, 0:1], qkg[:, 0:1], float(32 ** -0.5), None, ALU.mult, (B_qkg,), (B_qkgs,))
        T.copy(qkgs[:, 1:2], qkg[:, 1:2], (B_qkg,), (B_qkgs,))
        T.ts(subgb[:], subgb[:], float(8.0 * (1.0 - lam_init)), None, ALU.mult, (B_subg,), (B_subg,))
        T.memset(lamt[:], 0.0, (B_lamt,))
        T.tt(lam4b[:, 0, :], lam4b[:, 0, :], lam4b[:, 1, :], ALU.mult, (B_lam4,), (B_lam4,))
        T.tt(lam4b[:, 2, :], lam4b[:, 2, :], lam4b[:, 3, :], ALU.mult, (B_lam4,), (B_lam4,))
        T.rsum(lamt[:, 0:1], lam4b[:, 0, :], (B_lam4,), (B_lamt,))
        T.rsum(lamt[:, 1:2], lam4b[:, 2, :], (B_lam4,), (B_lamt,))
        T.act(lamt[:, 2:4], lamt[:, 0:2], AF.Exp, (B_lamt,), (B_lamt,))
        T.tt(lamt[:, 4:5], lamt[:, 3:4], lamt[:, 2:3], ALU.subtract, (B_lamt,), (B_lamt,))
        T.ts(lamt[:, 5:6], lamt[:, 4:5], float(-lam_init), None, ALU.add, (B_lamt,), (B_lamt,))
        nlam = lamt[:, 5:6]

        m0 = A.mark()
        wm = [A.t(f"wm{i}", [128, 8, 768], F32) for i in range(2)]
        for jg in range(8):
            wt, wb = wm[jg % 2]
            T.load("sp", wt[:], IN["w_mod"][l, :, jg * 768:(jg + 1) * 768].rearrange("(k p) j -> p k j", p=128), wb)
            for jj in range(6):
                j = jg * 6 + jj
                for k in range(8):
                    T.mm(ps[0][:, j * 3:(j + 1) * 3], wt[:, k, jj * 128:(jj + 1) * 128], sc[:, k, :],
                         k == 0, k == 7, (wb, B_sc), (PS[0],))
        T.tt(modT[:].rearrange("p j c -> p (j c)"), ps[0][:, 0:144], bmodx[:].rearrange("p j c -> p (j c)"),
             ALU.add, (PS[0], B_bmodx), (B_mod,))
        T.stt(gm[:, 0:8, :], modT[:, 8:16, :], 1.0, ngx[:, 0:8, :], ALU.add, ALU.mult, (B_mod, B_ngx), (B_gm,))
        T.stt(gm[:, 8:16, :], modT[:, 32:40, :], 1.0, ngx[:, 8:16, :], ALU.add, ALU.mult, (B_mod, B_ngx), (B_gm,))
        A.reset(m0)
        dump(f"modT{l}", modT[:], B_mod, [128, 48, 3])
        dump(f"gm{l}", gm[:], B_gm, [128, 16, 3])
        dump(f"lamt{l}", lamt[:], B_lamt, [128, 8])
        if stop == "mod":
            break

        def rms_modulate(xc, B_xc, N, col, which, outs, B_out, sq, B_sq, rstd, B_rstd, tmp, B_tmp, ssb):
            go = 0 if which == 1 else 8
            so = 0 if which == 1 else 24
            for k in range(8):
                q, bq = sq[k % 2], B_sq[k % 2]
                T.act(q[:, 0:N], xc[:, k, 0:N], AF.Square, (B_xc,), (bq,))
                T.mm(ps[ssb][:, 0:N], ones_bf[:], q[:, 0:N], k == 0, k == 7, (B_ones, bq), (PS[ssb],))
            T.act(rstd[:, 0:N], ps[ssb][:, 0:N], AF.Sqrt, (PS[ssb], B_epsb), (B_rstd,), bias=epsb[:, 0:1], scale=1.0)
            T.recip(rstd[:, 0:N], rstd[:, 0:N], (B_rstd,), (B_rstd,))
            for k in range(8):
                t_, bt = tmp[k % 2], B_tmp[k % 2]
                T.stt(t_[:, 0:N], xc[:, k, 0:N], gm[:, go + k, col:col + 1], rstd[:, 0:N], ALU.mult, ALU.mult,
                      (B_xc, B_gm, B_rstd), (bt,))
                T.act(outs[k], t_[:, 0:N], AF.Identity, (bt, B_mod), (B_out,), bias=modT[:, so + k, col:col + 1], scale=1.0)

        def qk_post(psb, N, gcol, is_ctx, t0, dst, B_dst, W):
            sqk, B_sqk, ug, B_ug, rstd2, B_rstd2, t1, B_t1, t2, B_t2, ropc, rops, B_rope = W
            T.act(sqk[:, 0:N], ps[psb][:, 0:N], AF.Square, (PS[psb],), (B_sqk,))
            T.ts(ug[:, 0:N], ps[psb][:, 0:N], qkgs[:, gcol:gcol + 1], None, ALU.mult, (PS[psb], B_qkgs), (B_ug,))
            T.mm(ps[3][:, 0:N], blk_bf[:], sqk[:, 0:N], True, True, (B_blk, B_sqk), (PS[3],))
            if not is_ctx:
                T.mm(ps[4][:, 0:N], perm_bf[:], ug[:, 0:N], True, True, (B_perm, B_ug), (PS[4],))
            T.act(rstd2[:, 0:N], ps[3][:, 0:N], AF.Sqrt, (PS[3], B_epsb), (B_rstd2,), bias=epsb[:, 0:1], scale=1.0)
            T.recip(rstd2[:, 0:N], rstd2[:, 0:N], (B_rstd2,), (B_rstd2,))
            if is_ctx:
                T.tt(dst, ug[:, 0:N], rstd2[:, 0:N], ALU.mult, (B_ug, B_rstd2), (B_dst,))
            else:
                T.tt(t1[:, 0:N], ug[:, 0:N], ropc[:, 0:N], ALU.mult, (B_ug, B_rope), (B_t1,))
                T.tt(t2[:, 0:N], ps[4][:, 0:N], rops[:, 0:N], ALU.mult, (PS[4], B_rope), (B_t2,))
                T.tt(t1[:, 0:N], t1[:, 0:N], t2[:, 0:N], ALU.add, (B_t1, B_t2), (B_t1,))
                T.tt(dst, t1[:, 0:N], rstd2[:, 0:N], ALU.mult, (B_t1, B_rstd2), (B_dst,))

        for s in range(NSEQ):
            A.reset(m0)
            KT, B_KT = A.t("KT", [128, 4, CL + L], BF16)
            VA, B_VA = A.t("VA", [128, (CL + L) // 128, 8, 65], BF16)
            T.memset(VA[:, :, :, 64:65], 1.0, (B_VA,))
            m1 = A.mark()
            wkv, B_wkv = A.t("wkv", [128, 8, 2048], BF16)
            for k in range(8):
                pass
            T.load("pool", wkv[:], IN["w_in"][l, :, 512:IN_W].rearrange("(k p) f -> p k f", p=128), B_wkv)
            xcs = [A.t(f"xc{i}", [128, 8, 512], F32) for i in range(2)]
            hT, B_hT = A.t("hT", [128, 8, 512], BF16)
            sq = [A.t(f"sq{i}", [128, 512], BF16) for i in range(2)]
            tmp = [A.t(f"tmp{i}", [128, 512], F32) for i in range(2)]
            rstd, B_rstd = A.t("rstd", [128, 512], F32)
            sqk, B_sqk = A.t("sqk", [128, 512], BF16)
            ug, B_ug = A.t("ug", [128, 512], BF16)
            rstd2, B_rstd2 = A.t("rstd2", [128, 512], F32)
            t1, B_t1 = A.t("t1", [128, 512], F32)
            t2, B_t2 = A.t("t2", [128, 512], F32)
            ropc, B_rope = A.t("ropc", [128, 512], F32)
            rops, _ = A.t("rops", [128, 512], F32)
            tC, B_tC = A.t("tC", [128, 512], F32)
            sts = [A.t(f"st{i}", [128, 6, 512], F32) for i in range(2)]
            W = (sqk, B_sqk, ug, B_ug, rstd2, B_rstd2, t1, B_t1, t2, B_t2, ropc, rops, B_rope)

            def load_chunk(ci):
                is_ctx, t0, N, koff = CHUNKS[ci]
                xc, bx = xcs[ci % 2]
                src = cin[s, :, :] if is_ctx else xin[s, :, t0:t0 + N]
                T.load("sp", xc[:, :, 0:N], src.rearrange("(k p) t -> p k t", p=128), bx)

            load_chunk(0)
            for ci, (is_ctx, t0, N, koff) in enumerate(CHUNKS):
                if p1chunks is not None and ci >= p1chunks:
                    break
                if ci + 1 < len(CHUNKS):
                    load_chunk(ci + 1)
                xc, bx = xcs[ci % 2]
                col = 2 if is_ctx else s
                if not is_ctx:
                    T.load("sp", ropc[:, 0:N], IN["ropeC"][:, t0:t0 + N], B_rope)
                    T.load("sp", rops[:, 0:N], IN["ropeS"][:, t0:t0 + N], B_rope)
                rms_modulate(xc[:], bx, N, col, 1, [hT[:, k, 0:N] for k in range(8)], B_hT,
                             [q[0] for q in sq], [q[1] for q in sq], rstd, B_rstd,
                             [q[0] for q in tmp], [q[1] for q in tmp], 0)
                for fc in range(4 if p1cut >= 2 else 0):
                    pb = 1 + fc % 2
                    for k in range(8):
                        T.mm(ps[pb][:, 0:N], wkv[:, k, fc * 128:(fc + 1) * 128], hT[:, k, 0:N], k == 0, k == 7,
                             (B_wkv, B_hT), (PS[pb],))
                    qk_post(pb, N, 1, is_ctx, t0, KT[:, fc, koff:koff + N], B_KT, W)
                for j in range(N // 128 if p1cut >= 3 else 0):
                    pb = 1 + j % 2
                    for k in range(8):
                        T.mm(ps[pb][:, 0:512], hT[:, k, j * 128:(j + 1) * 128], wkv[:, k, 512:1024], k == 0, k == 7,
                             (B_hT, B_wkv), (PS[pb],))
                    kt = koff // 128 + j
                    T.act(VA[:, kt, :, 0:64], ps[pb][:, 0:512].rearrange("p (h d) -> p h d", h=8), AF.Copy,
                          (PS[pb],), (B_VA,))
                st, bst = sts[ci % 2]

                def proj(colbase, c, pb):
                    for k in range(8):
                        T.mm(ps[pb][:, 0:N], wkv[:, k, colbase + c * 128:colbase + (c + 1) * 128], hT[:, k, 0:N],
                             k == 0, k == 7, (B_wkv, B_hT), (PS[pb],))
                if p1cut < 4:
                    continue
                for c in range(2):
                    proj(1024, c, 1)
                    T.act(st[:, c, 0:N], ps[1][:, 0:N], AF.Copy, (PS[1],), (bst,))
                    proj(1280, c, 2)
                    T.act(tC[:, 0:N], ps[2][:, 0:N], AF.Copy, (PS[2],), (B_tC,))
                    proj(1536, c, 1)
                    T.tt(st[:, 2 + c, 0:N], tC[:, 0:N], ps[1][:, 0:N], ALU.mult, (B_tC, PS[1]), (bst,))
                    proj(1792, c, 2)
                    T.act(st[:, 4 + c, 0:N], ps[2][:, 0:N], AF.Copy, (PS[2],), (bst,))
                T.store("sp", stash[s, :, :, koff:koff + N].rearrange("c p t -> p c t"), st[:, :, 0:N], bst)
            dump(f"KT{l}{s}", KT[:], B_KT, [128, 4, CL + L], BF16)
            dump(f"VA{l}{s}", VA[:], B_VA, [128, (CL + L) // 128, 8, 65], BF16)
            T.barrier()
            if stop == "p1":
                break

            A.reset(m1)
            wq, B_wq = A.t("wq", [128, 8, 512], BF16)
            wo, B_wo = A.t("wo", [128, 8, D], BF16)
            T.load("pool", wq[:], IN["w_in"][l, :, 0:512].rearrange("(k p) f -> p k f", p=128), B_wq)
            T.load("pool", wo[:], IN["w_out"][l].rearrange("(k p) f -> p k f", p=128), B_wo)
            xc, B_xc = A.t("xc", [128, 8, 512], F32)
            hT, B_hT = A.t("hT", [128, 8, 512], BF16)
            sq = [A.t(f"sq{i}", [128, 512], BF16) for i in range(2)]
            tmp = [A.t(f"tmp{i}", [128, 512], F32) for i in range(2)]
            rstd, B_rstd = A.t("rstd", [128, 512], F32)
            sqk, B_sqk = A.t("sqk", [128, 512], BF16)
            ug, B_ug = A.t("ug", [128, 512], BF16)
            rstd2, B_rstd2 = A.t("rstd2", [128, 512], F32)
            t1, B_t1 = A.t("t1", [128, 512], F32)
            mt2 = A.mark()
            pt2_alias, _ = A.t("t2", [128, 1024], BF16)
            A.reset(mt2)
            t2, B_t2 = A.t("t2", [128, 512], F32)
            ropc, B_rope = A.t("ropc", [128, 512], F32)
            rops, _ = A.t("rops", [128, 512], F32)
            W = (sqk, B_sqk, ug, B_ug, rstd2, B_rstd2, t1, B_t1, t2, B_t2, ropc, rops, B_rope)
            QT, B_QT = A.t("QT", [128, 4, 512], BF16)
            PT = [A.t(f"PT{i}", [128, 1024], BF16) for i in range(2)]
            PT.append((pt2_alias, B_t2))
            mixed, B_mixed = A.t("mixed", [128, 8, 512], BF16)
            rs, B_rs = A.t("rs", [128, 8], F32)
            o1, B_o1 = A.t("o1", [128, 64], F32)
            ocat, B_ocat = A.t("ocat", [128, 4, 64], F32)
            osq, B_osq = A.t("osq", [128, 4, 64], F32)
            oss, B_oss = A.t("oss", [128, 8], F32)
            onf = [[A.t(f"onf{a}{b}", [128, 128], F32) for b in range(4)] for a in range(2)]
            acnt = [0]
            sB, B_sB = A.t("sB", [128, 2, 512], F32)
            sZ, B_sZ = A.t("sZ", [128, 2, 514], F32)
            sU, B_sU = A.t("sU", [128, 2, 528], F32)
            pa, B_pa = A.t("pa", [128, 528], F32)
            pb_, B_pb = A.t("pb", [128, 528], F32)
            pd, B_pd = A.t("pd", [128, 512], BF16)
            cv, B_cv = t1, B_t1
            h2f, B_h2f = A.t("h2f", [128, 8, 512], F32)
            h2b, B_h2b = hT, B_hT
            lg, B_lg = A.t("lg", [128, NE], F32)
            m8, B_m8 = A.t("m8", [128, 8], F32)
            msk, B_msk = A.t("msk", [128, NE], F32)
            ex, B_ex = A.t("ex", [128, NE], F32)
            rt, B_rt = A.t("rt", [128, 4], F32)
            Gt, B_Gt = A.t("Gt", [128, NE], F32)
            GTc, B_GTc = A.t("GTc", [NE, 512], F32)

            chunks = CHUNKS if not last else CHUNKS[1:]
            if p2chunks is not None:
                chunks = chunks[:p2chunks]
            for (is_ctx, t0, N, koff) in chunks:
                col = 2 if is_ctx else s
                Lq = CL if is_ctx else L
                src = cin[s, :, :] if is_ctx else xin[s, :, t0:t0 + N]
                T.load("sp", xc[:, :, 0:N], src.rearrange("(k p) t -> p k t", p=128), B_xc)
                if not is_ctx:
                    T.load("sp", ropc[:, 0:N], IN["ropeC"][:, t0:t0 + N], B_rope)
                    T.load("sp", rops[:, 0:N], IN["ropeS"][:, t0:t0 + N], B_rope)
                T.load("sp", sB[:, :, 0:N], stash[s, 0:2, :, koff:koff + N].rearrange("c p t -> p c t"), B_sB)
                lo = 1 if t0 == 0 else 0
                hi = 1 if t0 + N == Lq else 0
                if lo:
                    T.memset(sZ[:, :, 0:1], 0.0, (B_sZ,))
                    T.memset(sU[:, :, 0:8], 0.0, (B_sU,))
                if hi:
                    T.memset(sZ[:, :, N + 1:N + 2], 0.0, (B_sZ,))
                    T.memset(sU[:, :, N + 8:N + 16], 0.0, (B_sU,))
                T.load("sp", sZ[:, :, lo:N + 2 - hi],
                       stash[s, 2:4, :, koff - 1 + lo:koff + N + 1 - hi].rearrange("c p t -> p c t"), B_sZ)
                T.load("sp", sU[:, :, 8 * lo:N + 16 - 8 * hi],
                       stash[s, 4:6, :, koff - 8 + 8 * lo:koff + N + 8 - 8 * hi].rearrange("c p t -> p c t"), B_sU)

                rms_modulate(xc[:], B_xc, N, col, 1, [hT[:, k, 0:N] for k in range(8)], B_hT,
                             [q[0] for q in sq], [q[1] for q in sq], rstd, B_rstd,
                             [q[0] for q in tmp], [q[1] for q in tmp], 0)
                for fc in range(4):
                    pbk = 1 + fc % 2
                    for k in range(8):
                        T.mm(ps[pbk][:, 0:N], wq[:, k, fc * 128:(fc + 1) * 128], hT[:, k, 0:N], k == 0, k == 7,
                             (B_wq, B_hT), (PS[pbk],))
                    qk_post(pbk, N, 0, is_ctx, t0, QT[:, fc, 0:N], B_QT, W)

                nk = (CL // 128) if is_ctx else (CL + L) // 128
                nqt = N // 128
                pend = None

                def emit_transposes(hp_, set_):
                    for qt in range(nqt):
                        tb = 4 + qt % 2
                        T.tr(ps[tb][:, 0:128], onf[set_][qt][0][:], ident[:], (onf[set_][qt][1], B_ident), (PS[tb],))
                        T.copy(mixed[:, hp_, qt * 128:(qt + 1) * 128], ps[tb][:, 0:128], (PS[tb],), (B_mixed,))

                for hp in range(4 if p2cut >= 2 else 0):
                    oset = hp % 2
                    for hl in range(2):
                        h = 2 * hp + hl
                        accb = (6, 7)

                        def scores(kt):
                            for c in range(2):
                                bank = 2 * (kt % 3) + c
                                r0 = 64 * hl + 32 * c
                                T.mm(ps[bank][:, 0:N], KT[r0:r0 + 32, hp, kt * 128:(kt + 1) * 128],
                                     QT[r0:r0 + 32, hp, 0:N], True, True, (B_KT, B_QT), (PS[bank],),
                                     tile_position=(r0, 0))
                        scores(0)
                        if nk > 1:
                            scores(1)
                        for kt in range(nk):
                            if kt + 2 < nk:
                                scores(kt + 2)
                            pt, bpt = PT[kt % 3]
                            for c in range(2):
                                bank = 2 * (kt % 3) + c
                                T.act(pt[:, c * 512:c * 512 + N], ps[bank][:, 0:N], AF.Exp, (PS[bank],), (bpt,))
                            for qt in range(nqt):
                                for c in range(2):
                                    bank = accb[qt // 2]
                                    ccol = ((qt % 2) * 2 + c) * 65
                                    T.mm(ps[bank][:, ccol:ccol + 65],
                                         pt[:, c * 512 + qt * 128:c * 512 + qt * 128 + 128], VA[:, kt, h, :],
                                         kt == 0 and qt % 2 == 0 and c == 0, kt == nk - 1, (bpt, B_VA), (PS[bank],),
                                         skip_group_check=True)
                        for qt in range(nqt):
                            bank = accb[qt // 2]
                            base = (qt % 2) * 130
                            acc = ps[bank][:, base:base + 130].rearrange("p (c d) -> p c d", c=2)
                            BA = PS[bank]
                            T.recip(rs[:, 0:2], acc[:, :, 64], (BA,), (B_rs,))
                            T.ts(rs[:, 2:3], rs[:, 1:2], nlam, None, ALU.mult, (B_rs, B_lamt), (B_rs,))
                            T.ts(o1[:], acc[:, 0, 0:64], rs[:, 0:1], None, ALU.mult, (BA, B_rs), (B_o1,))
                            T.stt(ocat[:, qt, :], acc[:, 1, 0:64], rs[:, 2:3], o1[:], ALU.mult, ALU.add,
                                  (BA, B_rs, B_o1), (B_ocat,))
                        T.tt(osq[:, 0:nqt, :], ocat[:, 0:nqt, :], ocat[:, 0:nqt, :], ALU.mult, (B_ocat,), (B_osq,))
                        T.rsum(oss[:, 0:nqt], osq[:, 0:nqt, :], (B_osq,), (B_oss,))
                        T.act(oss[:, 4:4 + nqt], oss[:, 0:nqt], AF.Ln, (B_oss, B_epsb), (B_oss,), bias=epsb[:, 1:2], scale=1.0)
                        T.act(oss[:, 4:4 + nqt], oss[:, 4:4 + nqt], AF.Exp, (B_oss,), (B_oss,), scale=-0.5)
                        for qt in range(nqt):
                            of, bof = onf[oset][qt]
                            T.stt(of[:, hl * 64:(hl + 1) * 64], ocat[:, qt, :], oss[:, 4 + qt:5 + qt],
                                  subgb[:, hl * 64:(hl + 1) * 64], ALU.mult, ALU.mult, (B_ocat, B_oss, B_subg), (bof,))
                        if hl == 0 and pend is not None:
                            emit_transposes(*pend)
                            pend = None
                    pend = (hp, oset)
                if pend is not None:
                    emit_transposes(*pend)

                if p2cut < 4:
                    continue
                for c in range(2):
                    T.ts(cv[:, 0:N], sZ[:, c, 0:N], convw[:, c, 0:1], None, ALU.mult, (B_sZ, B_convw), (B_cv,))
                    T.stt(cv[:, 0:N], sZ[:, c, 1:N + 1], convw[:, c, 1:2], cv[:, 0:N], ALU.mult, ALU.add,
                          (B_sZ, B_convw, B_cv), (B_cv,))
                    T.stt(cv[:, 0:N], sZ[:, c, 2:N + 2], convw[:, c, 2:3], cv[:, 0:N], ALU.mult, ALU.add,
                          (B_sZ, B_convw, B_cv), (B_cv,))
                    T.tt(mixed[:, 4 + c, 0:N], cv[:, 0:N], sB[:, c, 0:N], ALU.mult, (B_cv, B_sB), (B_mixed,))
                M = N + 16
                for c in range(2):
                    u = sU[:, c, :]
                    T.tt(pa[:, 1:M], u[:, 0:M - 1], u[:, 1:M], ALU.add, (B_sU,), (B_pa,))
                    if c == 0:
                        T.tt(pb_[64:128, 2:M - 1], pa[64:128, 1:M - 2], pa[64:128, 3:M], ALU.add, (B_pa,), (B_pb,))
                        T.copy(pb_[0:64, 8:8 + N], pa[0:64, 8:8 + N], (B_pa,), (B_pb,))
                        fin = pb_
                        B_fin = B_pb
                    else:
                        T.tt(pb_[:, 2:M - 1], pa[:, 1:M - 2], pa[:, 3:M], ALU.add, (B_pa,), (B_pb,))
                        T.tt(pa[:, 4:M - 3], pb_[:, 2:M - 5], pb_[:, 6:M - 1], ALU.add, (B_pb,), (B_pa,))
                        T.tt(pb_[64:128, 8:M - 7], pa[64:128, 4:M - 11], pa[64:128, 12:M - 3], ALU.add, (B_pa,), (B_pb,))
                        T.copy(pb_[0:64, 8:8 + N], pa[0:64, 8:8 + N], (B_pa,), (B_pb,))
                        fin = pb_
                        B_fin = B_pb
                    T.ts(pa[:, 0:N], fin[:, 8:8 + N], pwinv[:, c:c + 1], None, ALU.mult, (B_fin, B_pwinv), (B_pa,))
                    if lo:
                        T.tt(pa[:, 0:8], fin[:, 8:16], pedge[:, c, 0:8], ALU.mult, (B_fin, B_pedge), (B_pa,))
                    if hi:
                        T.tt(pa[:, N - 8:N], fin[:, N:N + 8], pedge[:, c, 8:16], ALU.mult, (B_fin, B_pedge), (B_pa,))
                    T.tt(pd[:, 0:N], pa[:, 0:N], u[:, 8:8 + N], ALU.subtract, (B_pa, B_sU), (B_pd,))
                    T.mm(ps[7][:, 0:N], poolbd[:, c, :], pd[:, 0:N], True, True, (B_poolbd, B_pd), (PS[7],))
                    T.ts(mixed[:, 6 + c, 0:N], ps[7][:, 0:N], pools[:, c:c + 1], None, ALU.mult, (PS[7], B_pools), (B_mixed,))
                if p2cut < 5:
                    continue
                for dc in range(8):
                    pbk = 6 + dc % 2
                    for k in range(8):
                        T.mm(ps[pbk][:, 0:N], wo[:, k, dc * 128:(dc + 1) * 128], mixed[:, k, 0:N], k == 0, k == 7,
                             (B_wo, B_mixed), (PS[pbk],))
                    T.stt(xc[:, dc, 0:N], ps[pbk][:, 0:N], modT[:, 16 + dc, col:col + 1], xc[:, dc, 0:N],
                          ALU.mult, ALU.add, (PS[pbk], B_mod, B_xc), (B_xc,))
                dst = c1res[s, :, :] if is_ctx else x1res[s, :, t0:t0 + N]
                T.store("sp", dst.rearrange("(k p) t -> p k t", p=128), xc[:, :, 0:N], B_xc)
                if p2cut < 6:
                    continue
                rms_modulate(xc[:], B_xc, N, col, 2, [h2f[:, k, 0:N] for k in range(8)], B_h2f,
                             [q[0] for q in sq], [q[1] for q in sq], rstd, B_rstd,
                             [q[0] for q in tmp], [q[1] for q in tmp], 0)
                T.copy(h2b[:, :, 0:N], h2f[:, :, 0:N], (B_h2f,), (B_h2b,))
                tok0 = (s * L + t0) if not is_ctx else (NSEQ * L + s * CL)
                T.store("sp", h2T[:, tok0:tok0 + N].rearrange("(k p) t -> p k t", p=128), h2b[:, :, 0:N], B_h2b)
                for j in range(N // 128):
                    for k in range(8):
                        T.mm(ps[1][:, 0:NE], h2f[:, k, j * 128:(j + 1) * 128], wr[:, k, :], k == 0, k == 7,
                             (B_h2f, B_wr), (PS[1],))
                    T.tt(lg[:], ps[1][:, 0:NE], rbb[:], ALU.add, (PS[1], B_rbb), (B_lg,))
                    T.max8(m8[:], lg[:], (B_lg,), (B_m8,))
                    T.ts(msk[:], lg[:], m8[:, 3:4], None, ALU.is_ge, (B_lg, B_m8), (B_msk,))
                    T.ts(rt[:, 0:1], m8[:, 0:1], -1.0, None, ALU.mult, (B_m8,), (B_rt,))
                    T.act(ex[:], lg[:], AF.Exp, (B_lg, B_rt), (B_ex,), bias=rt[:, 0:1], scale=1.0)
                    T.tt(ex[:], ex[:], msk[:], ALU.mult, (B_ex, B_msk), (B_ex,))
                    T.rsum(rt[:, 1:2], ex[:], (B_ex,), (B_rt,))
                    T.recip(rt[:, 2:3], rt[:, 1:2], (B_rt,), (B_rt,))
                    T.ts(Gt[:], ex[:], rt[:, 2:3], None, ALU.mult, (B_ex, B_rt), (B_Gt,))
                    T.tr(ps[2][0:NE, 0:128], Gt[:], ident[:], (B_Gt, B_ident), (PS[2],))
                    T.act(GTc[:, j * 128:(j + 1) * 128], ps[2][0:NE, 0:128], AF.Copy, (PS[2],), (B_GTc,))
                T.store("sp", GT[:, tok0:tok0 + N], GTc[:, 0:N], B_GTc)
            T.barrier()
            if stop == "p2":
                break
        if stop in ("p1", "p2", "p2all"):
            break

        A.reset(m0)
        Ttok = NSEQ * L + (0 if last else NSEQ * CL)
        wgu = [A.t(f"wgu{i}", [128, 8, 2 * D], BF16) for i in range(2)]
        wdn = [A.t(f"wdn{i}", [128, 8, D], BF16) for i in range(2)]
        mh = A.mark()
        x1c, B_x1c = A.t("h2g", [128, 8, 512], F32)
        A.reset(mh)
        h2g, B_h2g = A.t("h2g", [128, 8, TG], BF16)
        assert B_x1c is B_h2g
        GTg, B_GTg = A.t("GTg", [NE, TG], F32)
        yacc, B_yacc = A.t("yacc", [128, 8, TG], F32)
        actT = [A.t(f"actT{i}", [128, 8, 512], BF16) for i in range(2)]
        gbc = [A.t(f"gbc{i}", [128, 512], F32) for i in range(2)]
        g1s = [A.t(f"g1_{i}", [128, 512], F32) for i in range(2)]
        sgs = [A.t(f"sg_{i}", [128, 512], F32) for i in range(2)]
        l1s = [A.t(f"l1_{i}", [128, 512], F32) for i in range(2)]
        ngroups = (Ttok + TG - 1) // TG
        itc = [0]
        for g in range(ngroups):
            g0 = g * TG
            gn = min(TG, Ttok - g0)
            ntc = gn // 512
            T.load("sp", h2g[:, :, 0:gn], h2T[:, g0:g0 + gn].rearrange("(k p) t -> p k t", p=128), B_h2g)
            T.load("sp", GTg[:, 0:gn], GT[:, g0:g0 + gn], B_GTg)
            for tc in range(ntc):
                for dc in range(8):
                    pbk = 4 + dc % 2
                    T.mm(ps[pbk][:, :], bdn[:, dc * 128:(dc + 1) * 128], GTg[:, tc * 512:(tc + 1) * 512], True, True,
                         (B_bdn, B_GTg), (PS[pbk],))
                    T.act(yacc[:, dc, tc * 512:(tc + 1) * 512], ps[pbk][:, :], AF.Copy, (PS[pbk],), (B_yacc,))
            units = [(e_, tc) for e_ in range(ne_run) for tc in range(ntc)]
            ustate = {}

            def unit_setup(u):
                e_, tc = u
                if tc == 0:
                    wg, bwg = wgu[e_ % 2]
                    wd, bwd = wdn[e_ % 2]
                    T.load("pool", wg[:], IN["w_gate_up"][l, e_].rearrange("(k p) f -> p k f", p=128), bwg)
                    T.load("pool", wd[:], IN["w_down"][l, e_].rearrange("(k p) f -> p k f", p=128), bwd)
                gb, bgb = gbc[itc[0] % 2]
                at, bat = actT[itc[0] % 2]
                itc[0] += 1
                T.load("sp", gb[:], GT[e_, g0 + tc * 512:g0 + (tc + 1) * 512].partition_broadcast(128), bgb)
                ustate[u] = (gb, bgb, at, bat)

            def emit_gl(u, fcs):
                e_, tc = u
                if u not in ustate:
                    unit_setup(u)
                gb, bgb, at, bat = ustate[u]
                wg, bwg = wgu[e_ % 2]
                rhs_t = slice(tc * 512, (tc + 1) * 512)
                for fc in fcs:
                    pA = fc % 2
                    pB = 2 + fc % 2
                    g1, B_g1 = g1s[fc % 2]
                    sg, B_sg = sgs[fc % 2]
                    l1, B_l1 = l1s[fc % 2]
                    for k in range(8):
                        T.mm(ps[pA][:, :], wg[:, k, fc * 128:(fc + 1) * 128], h2g[:, k, rhs_t], k == 0, k == 7,
                             (bwg, B_h2g), (PS[pA],))
                    for k in range(8):
                        T.mm(ps[pB][:, :], wg[:, k, D + fc * 128:D + (fc + 1) * 128], h2g[:, k, rhs_t], k == 0, k == 7,
                             (bwg, B_h2g), (PS[pB],))
                    T.ts(g1[:], ps[pA][:, :], bgu[:, e_, fc:fc + 1], 7.0, ALU.add, (PS[pA], B_bgu), (B_g1,), op1=ALU.min)
                    T.act(sg[:], g1[:], AF.Sigmoid, (B_g1,), (B_sg,), scale=1.702)
                    T.ts(l1[:], ps[pB][:, :], bgu[:, e_, 8 + fc:9 + fc], 8.0, ALU.add, (PS[pB], B_bgu), (B_l1,), op1=ALU.min)
                    T.stt(l1[:], l1[:], -6.0, gb[:], ALU.max, ALU.mult, (B_l1, bgb), (B_l1,))
                    T.tt(g1[:], g1[:], sg[:], ALU.mult, (B_g1, B_sg), (B_g1,))
                    T.tt(at[:, fc, :], g1[:], l1[:], ALU.mult, (B_g1, B_l1), (bat,))

            def emit_down(u):
                e_, tc = u
                gb, bgb, at, bat = ustate.pop(u)
                wd, bwd = wdn[e_ % 2]
                rhs_t = slice(tc * 512, (tc + 1) * 512)
                for dc in range(8):
                    pY = 4 + dc % 2
                    for k in range(8):
                        T.mm(ps[pY][:, :], wd[:, k, dc * 128:(dc + 1) * 128], at[:, k, :], k == 0, k == 7,
                             (bwd, bat), (PS[pY],))
                    T.tt(yacc[:, dc, rhs_t], yacc[:, dc, rhs_t], ps[pY][:, :], ALU.add, (B_yacc, PS[pY]), (B_yacc,))

            if units:
                emit_gl(units[0], range(8))
            for ui, u in enumerate(units):
                nxt = units[ui + 1] if ui + 1 < len(units) else None
                if nxt is not None:
                    emit_gl(nxt, range(0, 4))
                emit_down(u)
                if nxt is not None:
                    emit_gl(nxt, range(4, 8))
            for tc in range(ntc):
                tok = g0 + tc * 512
                if tok < NSEQ * L:
                    s_ = tok // L
                    t0 = tok % L
                    col = s_
                    srcs = [(x1res[s_, :, t0:t0 + 512], xout[s_, :, t0:t0 + 512], 0, 512)]
                else:
                    col = 2
                    srcs = [(c1res[s_, :, :], c2res[s_, :, :], s_ * CL, CL) for s_ in range(NSEQ)]
                for (sap, dap, o, n) in srcs:
                    T.load("sp", x1c[:, :, o:o + n], sap.rearrange("(k p) t -> p k t", p=128), B_x1c)
                for dc in range(8):
                    T.stt(x1c[:, dc, :], yacc[:, dc, tc * 512:(tc + 1) * 512], modT[:, 40 + dc, col:col + 1], x1c[:, dc, :],
                          ALU.mult, ALU.add, (B_yacc, B_mod, B_x1c), (B_x1c,))
                for (sap, dap, o, n) in srcs:
                    T.store("sp", dap.rearrange("(k p) t -> p k t", p=128), x1c[:, :, o:o + n], B_x1c)
        T.barrier()

    if depth == DEPTH and stop_ is None:
        B_fin = Buf("fincopy")
        for s in range(NSEQ):
            T.dma("sp", out[s], x2res[s], B_fin)
    T.emit(nc)
    nc._trk = T
    nc._in_names = list(IN.keys())
    nc._alloc_peak = A.peak
    return nc


_CACHE = {}


def kernel(**inputs):
    I = {k: np.asarray(v) for k, v in inputs.items()}
    S = _shared_inputs(I)
    if "nc" not in _CACHE:
        _CACHE["nc"] = build()
    nc = _CACHE["nc"]
    in_maps = []
    for core in range(NCORES):
        m = dict(S)
        m.update(_core_inputs(I, core))
        in_maps.append({k: m[k] for k in nc._in_names})
    res = run_bass_kernel_spmd(nc, in_maps, core_ids=list(range(NCORES)))
    outs = [np.transpose(np.asarray(r["outT"]), (0, 2, 1)) for r in res.results]
    return np.ascontiguousarray(np.concatenate(outs, axis=0), dtype=np.float32)
```
